# Optimizing a Trainium2 kernel written in Bass

```python
import math
import jax, jax.numpy as jnp
from jax import lax
import numpy as np

D_MODEL = 1024
BATCH = 8
SEQ = 4096
DEPTH = 2

D_MIX = D_MODEL
SGU_GROUPS = 8
SGU_GROUP_DIM = 64
SGU_WIDTH = SGU_GROUPS * SGU_GROUP_DIM
CHUNK = 128
MLA_HEADS = 4
QK_NOPE = 128
QK_ROPE = 64
QK_HEAD = QK_NOPE + QK_ROPE
V_HEAD = 128
MLA_WIDTH = MLA_HEADS * V_HEAD
Q_LORA = 256
KV_LORA = 128
D_IN = 2 * SGU_WIDTH + Q_LORA + KV_LORA + QK_ROPE
IN_SPLITS = (SGU_WIDTH, 2 * SGU_WIDTH, 2 * SGU_WIDTH + Q_LORA, 2 * SGU_WIDTH + Q_LORA + KV_LORA)
ROPE_BASE = 10000.0
Q_BLOCK = 128
D_FF = 2816
N_EXPERTS = 8
TOP_K = 2
D_FF_EXPERT = 2816
N_MOD = 6
EPS = 1e-6
N_DENSE_LAYERS = (DEPTH + 1) // 2
N_MOE_LAYERS = DEPTH // 2

kernel_name = "hybrid_sgu_mla_moe_adaln"


def rms_norm(x, g):
    xf = x.astype(jnp.float32)
    y = xf * lax.rsqrt(jnp.mean(xf * xf, axis=-1, keepdims=True) + EPS)
    return (y * g.astype(jnp.float32)).astype(x.dtype)


def layer_norm(x, g):
    xf = x.astype(jnp.float32)
    mu = jnp.mean(xf, axis=-1, keepdims=True)
    var = jnp.mean(jnp.square(xf - mu), axis=-1, keepdims=True)
    y = (xf - mu) * lax.rsqrt(var + EPS)
    return (y * g.astype(jnp.float32)).astype(x.dtype)


def modulate(h, shift, scale):
    return h * (1 + scale[:, None, :]) + shift[:, None, :]


def rope_tables(positions):
    inv_freq = 1.0 / (ROPE_BASE ** (jnp.arange(0, QK_ROPE, 2, dtype=jnp.float32) / QK_ROPE))
    ang = positions.astype(jnp.float32)[..., None] * inv_freq
    return jnp.cos(ang)[:, :, None, :], jnp.sin(ang)[:, :, None, :]


def rope_tail(x, cos, sin):
    x_nope, x_rope = jnp.split(x, [QK_NOPE], axis=-1)
    x1, x2 = jnp.split(x_rope, 2, axis=-1)
    cos = cos.astype(x.dtype)
    sin = sin.astype(x.dtype)
    return jnp.concatenate([x_nope, x1 * cos - x2 * sin, x2 * cos + x1 * sin], axis=-1)


def spatial_gating(zu, zv, norm_g, w_s, b_s):
    B, S, _ = zu.shape
    n = S // CHUNK
    u = jax.nn.gelu(zu)
    v = layer_norm(jax.nn.gelu(zv), norm_g)
    v = v.reshape(B, n, CHUNK, SGU_GROUPS, SGU_GROUP_DIM)
    w_causal = w_s * jnp.tril(jnp.ones((CHUNK, CHUNK), w_s.dtype))
    s = jnp.einsum('gts,bnsgd->bntgd', w_causal, v) + b_s.T[None, None, :, :, None]
    return u * s.reshape(B, S, SGU_WIDTH)


def causal_block_attention(q, k, v):
    B, S, H, Dq = q.shape
    nb = S // Q_BLOCK
    scale = Dq ** -0.5
    qb = q.reshape(B, nb, Q_BLOCK, H, Dq).transpose(1, 0, 3, 2, 4)
    kf = k.astype(jnp.float32)
    k_pos = jnp.arange(S)
    neg = jnp.finfo(jnp.float32).min

    def one_block(args):
        q_blk, blk = args
        s = jnp.einsum('bhqd,bkhd->bhqk', q_blk.astype(jnp.float32), kf) * scale
        q_pos = blk * Q_BLOCK + jnp.arange(Q_BLOCK)
        s = jnp.where(k_pos[None, :] <= q_pos[:, None], s, neg)
        p = jax.nn.softmax(s, axis=-1)
        return jnp.einsum('bhqk,bkhd->bqhd', p.astype(v.dtype), v)

    o = lax.map(one_block, (qb, jnp.arange(nb)))
    return o.transpose(1, 0, 2, 3, 4).reshape(B, S, H, V_HEAD)


def latent_attention(cq, ckv, k_rope, cos, sin, q_lat_norm, kv_lat_norm, w_uq, w_ukv, q_norm, k_norm):
    B, S, _ = cq.shape
    q = (rms_norm(cq, q_lat_norm) @ w_uq).reshape(B, S, MLA_HEADS, QK_HEAD)
    kv = (rms_norm(ckv, kv_lat_norm) @ w_ukv).reshape(B, S, MLA_HEADS, QK_NOPE + V_HEAD)
    k_nope, v = jnp.split(kv, [QK_NOPE], axis=-1)
    k_r = jnp.broadcast_to(k_rope[:, :, None, :], (B, S, MLA_HEADS, QK_ROPE))
    k = jnp.concatenate([k_nope, k_r], axis=-1)
    q = rope_tail(rms_norm(q, q_norm), cos, sin)
    k = rope_tail(rms_norm(k, k_norm), cos, sin)
    o = causal_block_attention(q, k, v)
    return o.reshape(B, S, MLA_WIDTH)


def hybrid_mixer(h, cos, sin, w_in, sgu_norm, sgu_w, sgu_b, q_lat_norm, kv_lat_norm,
                 w_uq, w_ukv, q_norm, k_norm, w_out):
    proj = h @ w_in
    zu, zv, cq, ckv, k_rope = jnp.split(proj, IN_SPLITS, axis=-1)
    a = spatial_gating(zu, zv, sgu_norm, sgu_w, sgu_b)
    m = latent_attention(cq, ckv, k_rope, cos, sin, q_lat_norm, kv_lat_norm,
                         w_uq, w_ukv, q_norm, k_norm)
    return jnp.concatenate([a, m], axis=-1) @ w_out


def swiglu(x, w1, w3, w2):
    return (jax.nn.silu(x @ w1) * (x @ w3)) @ w2


def moe_swiglu(h, router_w, w1, w3, w2):
    B, S, D = h.shape
    xt = h.reshape(-1, D)
    logits = (xt @ router_w).astype(jnp.float32)
    top_val, top_idx = lax.top_k(logits, TOP_K)
    top_w = jax.nn.softmax(top_val, axis=-1)
    gates = jnp.sum(jax.nn.one_hot(top_idx, N_EXPERTS, dtype=jnp.float32) * top_w[..., None],
                    axis=1).astype(h.dtype)
    y = jnp.zeros_like(xt)
    for e in range(N_EXPERTS):
        y = y + gates[:, e:e + 1] * swiglu(xt, w1[e], w3[e], w2[e])
    return y.reshape(B, S, D)


def setup_inputs(seed: int = 0) -> dict:
    key = jax.random.key(seed)
    ks = jax.random.split(key, 32)
    L = DEPTH
    f = jnp.float32

    def nrm(k, shape, scale):
        return jax.random.normal(k, shape, f) * scale

    def gain(k, shape):
        return 1.0 + 0.02 * jax.random.normal(k, shape, f)

    positions = (jnp.arange(SEQ, dtype=jnp.int32)[None, :]
                 + jax.random.randint(ks[2], (BATCH, 1), 0, 1024, dtype=jnp.int32))
    return {
        "x": nrm(ks[0], (BATCH, SEQ, D_MODEL), 1.0),
        "c": nrm(ks[1], (BATCH, D_MODEL), 1.0),
        "positions": positions,
        "ada_w": nrm(ks[3], (L, D_MODEL, N_MOD * D_MODEL), 0.5 * D_MODEL ** -0.5),
        "ada_b": nrm(ks[4], (L, N_MOD * D_MODEL), 0.01),
        "norm_mix": gain(ks[5], (L, D_MODEL)),
        "norm_ffn": gain(ks[6], (L, D_MODEL)),
        "w_in": nrm(ks[7], (L, D_MODEL, D_IN), D_MODEL ** -0.5),
        "sgu_norm": gain(ks[8], (L, SGU_WIDTH)),
        "sgu_w": nrm(ks[9], (L, SGU_GROUPS, CHUNK, CHUNK), CHUNK ** -0.5),
        "sgu_b": 1.0 + 0.1 * jax.random.normal(ks[10], (L, SGU_GROUPS, CHUNK), f),
        "q_lat_norm": gain(ks[11], (L, Q_LORA)),
        "kv_lat_norm": gain(ks[12], (L, KV_LORA)),
        "w_uq": nrm(ks[13], (L, Q_LORA, MLA_HEADS * QK_HEAD), Q_LORA ** -0.5),
        "w_ukv": nrm(ks[14], (L, KV_LORA, MLA_HEADS * (QK_NOPE + V_HEAD)), KV_LORA ** -0.5),
        "q_norm": gain(ks[15], (L, QK_HEAD)),
        "k_norm": gain(ks[16], (L, QK_HEAD)),
        "w_out": nrm(ks[17], (L, D_MIX, D_MODEL), D_MIX ** -0.5),
        "ffn_w1": nrm(ks[18], (N_DENSE_LAYERS, D_MODEL, D_FF), D_MODEL ** -0.5),
        "ffn_w3": nrm(ks[19], (N_DENSE_LAYERS, D_MODEL, D_FF), D_MODEL ** -0.5),
        "ffn_w2": nrm(ks[20], (N_DENSE_LAYERS, D_FF, D_MODEL), D_FF ** -0.5),
        "router_w": nrm(ks[21], (N_MOE_LAYERS, D_MODEL, N_EXPERTS), D_MODEL ** -0.5),
        "moe_w1": nrm(ks[22], (N_MOE_LAYERS, N_EXPERTS, D_MODEL, D_FF_EXPERT), D_MODEL ** -0.5),
        "moe_w3": nrm(ks[23], (N_MOE_LAYERS, N_EXPERTS, D_MODEL, D_FF_EXPERT), D_MODEL ** -0.5),
        "moe_w2": nrm(ks[24], (N_MOE_LAYERS, N_EXPERTS, D_FF_EXPERT, D_MODEL), D_FF_EXPERT ** -0.5),
    }


def reference(x, c, positions, ada_w, ada_b, norm_mix, norm_ffn, w_in, sgu_norm, sgu_w, sgu_b,
              q_lat_norm, kv_lat_norm, w_uq, w_ukv, q_norm, k_norm, w_out,
              ffn_w1, ffn_w3, ffn_w2, router_w, moe_w1, moe_w3, moe_w2):
    cos, sin = rope_tables(positions)
    c_act = jax.nn.silu(c)
    for layer in range(DEPTH):
        mod = c_act @ ada_w[layer] + ada_b[layer]
        sh1, sc1, g1, sh2, sc2, g2 = jnp.split(mod, N_MOD, axis=-1)
        h = modulate(rms_norm(x, norm_mix[layer]), sh1, sc1)
        y = hybrid_mixer(h, cos, sin, w_in[layer], sgu_norm[layer], sgu_w[layer], sgu_b[layer],
                         q_lat_norm[layer], kv_lat_norm[layer], w_uq[layer], w_ukv[layer],
                         q_norm[layer], k_norm[layer], w_out[layer])
        x = x + g1[:, None, :] * y
        h = modulate(rms_norm(x, norm_ffn[layer]), sh2, sc2)
        i = layer // 2
        if layer % 2 == 0:
            y = swiglu(h, ffn_w1[i], ffn_w3[i], ffn_w2[i])
        else:
            y = moe_swiglu(h, router_w[i], moe_w1[i], moe_w3[i], moe_w2[i])
        x = x + g2[:, None, :] * y
    return x
```

```python
import contextlib
import numpy as np
import concourse.bass as bass
import concourse.mybir as mybir
from concourse.bass_utils import run_bass_kernel_spmd

F32, BF16, I32 = mybir.dt.float32, mybir.dt.bfloat16, mybir.dt.int32
AF = mybir.ActivationFunctionType
ALU = mybir.AluOpType
AX = mybir.AxisListType

S = 4096
D = 1024
NT = S // 128
DFF = 2816
NF = DFF // 128
NE = 8
EPS = 1e-6
D_IN = 1472
ENGS = ("sp", "act", "dve", "pool", "pe")
_DT_SIZE = {F32: 4, BF16: 2, I32: 4}


class Buf:
    __slots__ = ("name", "w", "r", "dsem", "dcnt")

    def __init__(self, name):
        self.name = name
        self.w = None
        self.r = {}
        self.dsem = None
        self.dcnt = 0


class View:
    def __init__(self, ap, buf):
        self.ap = ap
        self.buf = buf

    def __getitem__(self, k):
        return View(self.ap[k], self.buf)

    def re(self, s, **kw):
        return View(self.ap.rearrange(s, **kw), self.buf)

    def bc(self, shape):
        return View(self.ap.to_broadcast(list(shape)), self.buf)

    def bitcast(self, dt):
        return View(self.ap.bitcast(dt), self.buf)

    def unsq(self, ax):
        return View(self.ap.unsqueeze(ax), self.buf)

    def on(self, buf):
        return View(self.ap, buf)


def _ap(v):
    return v.ap if isinstance(v, View) else v


class Prog:
    def __init__(self, nc, stack):
        self.nc = nc
        self.stack = stack
        self.q = {e: [] for e in ENGS}
        self.sem = {}
        self.cnt = {}
        self.waited = {e: {} for e in ENGS}
        self.nsem = 0
        for e in ENGS:
            self._new_sem(e)
        self.dma_events = {}
        self.sb_off = 16640
        self.sb_top = 229344
        self.ntile = 0

    def tile(self, shape, dt, name="t", nbuf=None):
        nbytes = int(np.prod(shape[1:])) * _DT_SIZE[dt]
        nbytes = (nbytes + 63) // 64 * 64
        off = self.sb_off
        self.sb_off += nbytes
        assert self.sb_off <= self.sb_top, f"SBUF overflow at {name}: {self.sb_off}"
        self.ntile += 1
        h = self.nc.alloc_sbuf_tensor_at(f"{name}_{self.ntile}", list(shape), dt, offset=off)
        return View(h.ap(), Buf(name))

    def psum(self, name):
        self.ntile += 1
        h = self.nc.alloc_psum_tensor(f"{name}_{self.ntile}", [128, 512], F32)
        return View(h.ap(), Buf(name))

    def _alloc_sem(self, name):
        self.nsem += 1
        return self.stack.enter_context(self.nc.semaphore(f"{name}_{self.nsem}"))

    def _new_sem(self, e):
        self.sem[e] = self._alloc_sem(f"s_{e}")
        self.cnt[e] = 0

    def wait(self, e, ev):
        sem, v = ev
        if self.waited[e].get(sem, 0) >= v:
            return
        self.waited[e][sem] = v
        self.q[e].append(lambda eng, sem=sem, v=v: eng.wait_ge(sem, v))

    def _deps(self, e, reads, writes):
        deps = {}

        def add(sem, v):
            if deps.get(sem, 0) < v:
                deps[sem] = v
        for b in reads:
            if b.w is not None:
                add(*b.w)
        for b in writes:
            if b.w is not None:
                add(*b.w)
            for sem, v in b.r.items():
                add(sem, v)
        for sem, v in deps.items():
            self.wait(e, (sem, v))

    def _record(self, ev, reads, writes):
        for b in reads:
            if b.r.get(ev[0], 0) < ev[1]:
                b.r[ev[0]] = ev[1]
        for b in writes:
            b.w = ev
            b.r = {}

    def emit(self, e, fn, reads=(), writes=()):
        reads = [b for b in reads if b is not None]
        writes = [b for b in writes if b is not None]
        self._deps(e, reads, writes)
        if self.cnt[e] >= 30000:
            self._new_sem(e)
        self.cnt[e] += 1
        ev = (self.sem[e], self.cnt[e])
        self.q[e].append(lambda eng, sem=ev[0]: fn(eng).then_inc(sem, 1))
        self._record(ev, reads, writes)
        return ev

    def emit_group(self, e, fns, reads, writes):
        self._deps(e, reads, writes)
        if self.cnt[e] >= 30000:
            self._new_sem(e)
        self.cnt[e] += 1
        ev = (self.sem[e], self.cnt[e])
        for fn in fns[:-1]:
            self.q[e].append(lambda eng, fn=fn: fn(eng))
        self.q[e].append(lambda eng, sem=ev[0], fn=fns[-1]: fn(eng).then_inc(sem, 1))
        self._record(ev, reads, writes)
        return ev

    def dma(self, e, out, in_, sb):
        b = sb.buf
        reads = [in_.buf] if isinstance(in_, View) else []
        writes = [out.buf] if isinstance(out, View) else []
        self._deps(e, reads, writes)
        if b.dsem is None:
            b.dsem = self._alloc_sem("d_" + b.name)
        b.dcnt += 16
        ev = (b.dsem, b.dcnt)
        o, i = _ap(out), _ap(in_)
        self.q[e].append(lambda eng, o=o, i=i, sem=ev[0]: eng.dma_start(out=o, in_=i).then_inc(sem, 16))
        self._record(ev, reads, writes)
        self.dma_events[ev[0]] = ev[1]
        return ev

    def idma(self, out, in_, idx, sb, scatter):
        e = "pool"
        b = sb.buf
        reads = [idx.buf] + ([in_.buf] if isinstance(in_, View) else [])
        writes = [out.buf] if isinstance(out, View) else []
        self._deps(e, reads, writes)
        if b.dsem is None:
            b.dsem = self._alloc_sem("d_" + b.name)
        b.dcnt += 16
        ev = (b.dsem, b.dcnt)
        o, i, ix = _ap(out), _ap(in_), idx.ap

        def th(eng, o=o, i=i, ix=ix, sem=ev[0], scatter=scatter):
            off = bass.IndirectOffsetOnAxis(ap=ix, axis=0)
            if scatter:
                ins = eng.indirect_dma_start(out=o, out_offset=off, in_=i, in_offset=None)
            else:
                ins = eng.indirect_dma_start(out=o, out_offset=None, in_=i, in_offset=off)
            ins.then_inc(sem, 16)
        self.q[e].append(th)
        self._record(ev, reads, writes)
        self.dma_events[ev[0]] = ev[1]
        return ev

    def barrier(self):
        evs = [(self.sem[e], self.cnt[e]) for e in ENGS if self.cnt[e] > 0]
        evs += list(self.dma_events.items())
        for e in ENGS:
            for ev in evs:
                self.wait(e, ev)
        self.dma_events = {}

    def act(self, out, in_, func, scale=1.0, bias=0.0, accum=None):
        reads = [in_.buf] + [v.buf for v in (scale, bias) if isinstance(v, View)]
        writes = [out.buf] + ([accum.buf] if accum is not None else [])
        kw = dict(out=out.ap, in_=in_.ap, func=func, scale=_ap(scale), bias=_ap(bias))
        if accum is not None:
            kw["accum_out"] = accum.ap
        return self.emit("act", lambda e: e.activation(**kw), reads, writes)

    def tt(self, out, a, b, op, eng="dve"):
        return self.emit(eng, lambda e: e.tensor_tensor(out=out.ap, in0=a.ap, in1=b.ap, op=op),
                         [a.buf, b.buf], [out.buf])

    def ts(self, out, a, s1, s2, op0, op1=None, eng="dve", accum=None):
        reads = [a.buf] + [v.buf for v in (s1, s2) if isinstance(v, View)]
        writes = [out.buf] + ([accum.buf] if accum is not None else [])
        kw = dict(out=out.ap, in0=a.ap, scalar1=_ap(s1), scalar2=_ap(s2), op0=op0)
        if op1 is not None:
            kw["op1"] = op1
        if accum is not None:
            kw["accum_out"] = accum.ap
        return self.emit(eng, lambda e: e.tensor_scalar(**kw), reads, writes)

    def stt(self, out, a, s, b, op0, op1, accum=None):
        reads = [a.buf, b.buf] + ([s.buf] if isinstance(s, View) else [])
        writes = [out.buf] + ([accum.buf] if accum is not None else [])
        kw = dict(out=out.ap, in0=a.ap, scalar=_ap(s), in1=b.ap, op0=op0, op1=op1)
        if accum is not None:
            kw["accum_out"] = accum.ap
        return self.emit("dve", lambda e: e.scalar_tensor_tensor(**kw), reads, writes)

    def copy(self, out, a, eng="dve"):
        if eng == "act":
            return self.emit("act", lambda e: e.copy(out=out.ap, in_=a.ap), [a.buf], [out.buf])
        return self.emit(eng, lambda e: e.tensor_copy(out=out.ap, in_=a.ap), [a.buf], [out.buf])

    def memset(self, out, val, eng="pool"):
        return self.emit(eng, lambda e: e.memset(out.ap, val), [], [out.buf])

    def reduce(self, out, a, op=ALU.add):
        return self.emit("dve", lambda e: e.tensor_reduce(out=out.ap, in_=a.ap, axis=AX.X, op=op),
                         [a.buf], [out.buf])

    def recip(self, out, a):
        return self.emit("dve", lambda e: e.reciprocal(out=out.ap, in_=a.ap), [a.buf], [out.buf])

    def rsqrt(self, out, a, nh):
        return self.emit("pool", lambda e: e.tensor_tensor(out=out.ap, in0=a.ap, in1=nh.ap, op=ALU.pow),
                         [a.buf, nh.buf], [out.buf])

    def pe(self, items):
        fns, reads, writes = [], [], []
        for it in items:
            if it[0] == "mm":
                _, out, l, r, st, sp = it
                fns.append(lambda e, out=out, l=l, r=r, st=st, sp=sp:
                           e.matmul(out.ap, l.ap, r.ap, start=st, stop=sp))
                reads += [l.buf, r.buf]
                writes.append(out.buf)
            else:
                _, out, a, idn = it
                fns.append(lambda e, out=out, a=a, idn=idn: e.transpose(out.ap, a.ap, idn.ap))
                reads += [a.buf, idn.buf]
                writes.append(out.buf)
        reads = list({id(b): b for b in reads}.values())
        writes = list({id(b): b for b in writes}.values())
        return self.emit_group("pe", fns, reads, writes)

    def finish(self):
        nc = self.nc
        q = self.q
        with nc.Block() as block:
            @block.sync
            def _(eng):
                for th in q["sp"]:
                    th(eng)

            @block.scalar
            def _(eng):
                for th in q["act"]:
                    th(eng)

            @block.vector
            def _(eng):
                for th in q["dve"]:
                    th(eng)

            @block.gpsimd
            def _(eng):
                for th in q["pool"]:
                    th(eng)

            @block.tensor
            def _(eng):
                for th in q["pe"]:
                    th(eng)


def build_program(stop_after=None, n_tiles_dbg=None):
    nc = bass.Bass("TRN2", target_bir_lowering=False)
    stack = contextlib.ExitStack()
    P = Prog(nc, stack)

    def din(name, shape, dt=F32):
        return nc.dram_tensor(name, list(shape), dt, kind="ExternalInput").ap()

    x_d = din("x", [S, D])
    cT_d = din("cT", [128, 8])
    pos_d = din("pos", [128, NT], I32)
    invf_d = din("invf", [128, 32])
    ident_d = din("ident", [128, 128])
    tri_d = din("tri", [128, 128])
    ada_w_d = din("ada_w", [2, D, 6 * D])
    ada_b_d = din("ada_b", [2, 6 * D])
    nmix_d = din("nmix_b", [2, 128, D])
    nffn_d = din("nffn_b", [2, 128, D])
    w_in_d = din("w_in", [2, D, D_IN])
    sgn_d = din("sgn_b", [2, 128, 512])
    sguT_d = din("sguT", [2, 128, 8, 128])
    sgb_d = din("sgb", [2, 128, 512])
    qln_d = din("qln_b", [2, 128, 256])
    kvln_d = din("kvln_b", [2, 128, 128])
    w_uq_d = din("w_uq", [2, 256, 768])
    w_ukv_d = din("w_ukv", [2, 128, 1024])
    qn_d = din("qn_b", [2, 128, 192])
    kn_d = din("kn_b", [2, 128, 192])
    w_out_d = din("w_out", [2, D, D])
    fw1L_d = din("fw1L", [11 * 128, 2048])
    fw3L_d = din("fw3L", [11 * 128, 2048])
    fw2L_d = din("fw2L", [128, NF * D])
    rw_d = din("router_w", [1, D, NE])
    SPARSE = True
    if SPARSE:
        w1L_d = din("w1L", [NE * 11 * 128, 2048])
        w3L_d = din("w3L", [NE * 11 * 128, 2048])
        w2L_d = din("w2L", [NE * 128 * 2, 11 * D])
        ebase_d = din("ebase", [128, NE])
        giota_d = din("giota", [128, 16])
        cth_d = din("ct_h", [128, 8])
        ctw_d = din("ct_w", [128, 11])
        ctw2_d = din("ct_w2", [128, 2])
        hs_d = nc.dram_tensor("hs", [NE * S, D], BF16, kind="Internal").ap()
        ys_d = nc.dram_tensor("ys", [NE * S, D], F32, kind="Internal").ap()
    else:
        moe_w1_d = din("moe_w1", [1, NE, D, DFF])
        moe_w3_d = din("moe_w3", [1, NE, D, DFF])
        moe_w2_d = din("moe_w2", [1, NE, DFF, D])
    y_d = nc.dram_tensor("y", [S, D], F32, kind="ExternalOutput").ap()
    xs_d = nc.dram_tensor("xs", [S, D], F32, kind="Internal").ap()

    nt_run = NT if n_tiles_dbg is None else n_tiles_dbg

    PS = [P.psum(f"ps{i}") for i in range(8)]

    ident = P.tile([128, 128], BF16, "ident")
    tri = P.tile([128, 128], BF16, "tri")
    ones = P.tile([128, 128], BF16, "ones")
    ident32 = P.tile([128, 128], F32, "ident32")
    nhalf = P.tile([128, 8], F32, "nhalf")
    cosT = P.tile([128, NT, 32], BF16, "cos")
    sinT = P.tile([128, NT, 32], BF16, "sin")
    cact = P.tile([128, 8], F32, "cact")
    ones1 = P.tile([1, 128], F32, "ones1")
    modb = P.tile([128, 3, D], BF16, "modb")
    persist_mark = P.sb_off

    P.dma("pool", ident, ident_d, ident)
    P.dma("pool", tri, tri_d, tri)
    P.dma("sp", ident32, ident_d, ident32)
    P.memset(ones, 1.0)
    P.memset(nhalf, -0.5)
    P.memset(ones1, 1.0)
    m0 = P.sb_off
    cT = P.tile([128, 8], F32, "cT")
    posi = P.tile([128, NT], I32, "posi")
    posf = P.tile([128, NT], F32, "posf")
    invf = P.tile([128, 32], F32, "invf")
    ang = P.tile([128, NT, 32], F32, "ang")
    ang2 = P.tile([128, NT, 32], F32, "ang2")
    P.dma("sp", cT, cT_d, cT)
    P.dma("sp", posi, pos_d, posi)
    P.dma("sp", invf, invf_d, invf)
    P.act(cact, cT, AF.Silu)
    P.copy(posf, posi)
    P.tt(ang, posf.unsq(2).bc([128, NT, 32]), invf.unsq(1).bc([128, NT, 32]), ALU.mult)
    angi = P.tile([128, NT, 32], I32, "angi")
    angf = P.tile([128, NT, 32], F32, "angf")
    msk = P.tile([128, NT, 32], F32, "msk")

    def sin_of(dst, shift):
        P.ts(ang2, ang, float(1.0 / (2.0 * np.pi)), float(shift), ALU.mult, ALU.add)
        P.copy(angi, ang2)
        P.copy(angf, angi)
        P.tt(ang2, ang2, angf, ALU.subtract)
        P.ts(msk, ang2, 0.5, None, ALU.is_gt)
        P.tt(ang2, ang2, msk, ALU.subtract)
        P.ts(msk, ang2, -0.5, None, ALU.is_lt)
        P.tt(ang2, ang2, msk, ALU.add)
        P.act(dst, ang2, AF.Sin, scale=6.283185)

    sin_of(sinT, 0.0)
    sin_of(cosT, 0.25)
    P.barrier()
    P.sb_off = m0

    def compute_mod(layer, sub, norm_d):
        m = P.sb_off
        wbuf = [P.tile([128, 3 * D], F32, f"adaw{i}") for i in range(2)]
        crep = P.tile([128, 8, 128], F32, "crep")
        for k in range(8):
            P.copy(crep[:, k, :], cact[:, k:k + 1].bc([128, 128]))
        brow = P.tile([1, 3 * D], F32, "brow")
        gb = P.tile([128, D], F32, "gb")
        t1 = P.tile([128, D], F32, "t1")
        c0 = sub * 3 * D
        P.dma("sp", brow, ada_b_d[layer:layer + 1, c0:c0 + 3 * D], brow)
        P.dma("sp", gb, norm_d[layer], gb)
        for k in range(8):
            wb = wbuf[k % 2]
            P.dma("sp", wb, ada_w_d[layer, k * 128:(k + 1) * 128, c0:c0 + 3 * D], wb)
            for j in range(6):
                P.pe([("mm", PS[j], crep[:, k, :], wb[:, j * 512:(j + 1) * 512], k == 0, False)])
        for j in range(6):
            P.pe([("mm", PS[j], ones1, brow[:, j * 512:(j + 1) * 512], False, True)])
        for hh in range(2):
            P.stt(t1[:, hh * 512:(hh + 1) * 512], PS[2 + hh], 1.0, gb[:, hh * 512:(hh + 1) * 512],
                  ALU.add, ALU.mult)
            P.copy(modb[:, 1, hh * 512:(hh + 1) * 512], PS[0 + hh], eng="act")
            P.copy(modb[:, 2, hh * 512:(hh + 1) * 512], PS[4 + hh], eng="act")
        P.copy(modb[:, 0, :], t1)
        P.barrier()
        P.sb_off = m

    def norm_tile(xt, hT_dst, tmp, h32=None):
        junk, ss, ms, rstd, hbf, tpsum = tmp
        P.act(junk, xt, AF.Square, accum=ss)
        P.ts(ms, ss, 1.0 / D, EPS, ALU.mult, ALU.add)
        P.rsqrt(rstd, ms, nhalf[:, 0:1])
        hdst = h32 if h32 is not None else junk
        P.stt(hdst, xt, rstd, modb[:, 0, :], ALU.mult, ALU.mult)
        if h32 is not None:
            P.tt(h32, h32, modb[:, 1, :], ALU.add)
            P.copy(hbf, h32, eng="act")
        else:
            P.tt(hbf, junk, modb[:, 1, :], ALU.add)
        tp = tpsum.bitcast(BF16)
        P.pe([("tr", tp[:, j * 128:(j + 1) * 128], hbf[:, j * 128:(j + 1) * 128], ident) for j in range(8)])
        P.copy(hT_dst, tp.re("p (k t) -> p k t", k=8), eng="act")

    def mixer(layer, src_d, dst_d):
        m = P.sb_off
        Win = P.tile([128, 8, D_IN], BF16, "Win")
        Wuq = P.tile([128, 2, 768], BF16, "Wuq")
        Wukv = P.tile([128, 1024], BF16, "Wukv")
        Wout = P.tile([128, 8, D], BF16, "Wout")
        WcT = P.tile([128, 8, 128], BF16, "WcT")
        sgn = P.tile([128, 512], BF16, "sgn")
        sgb = P.tile([128, 512], BF16, "sgb")
        qln = P.tile([128, 256], F32, "qln")
        kvln = P.tile([128, 128], F32, "kvln")
        qn = P.tile([128, 192], F32, "qn")
        kn = P.tile([128, 192], F32, "kn")
        GQ = P.tile([128, 128], F32, "GQ")
        P.dma("pool", Win, w_in_d[layer].rearrange("(k p) n -> p k n", p=128), Win)
        P.dma("pool", Wuq, w_uq_d[layer].rearrange("(k p) n -> p k n", p=128), Wuq)
        P.dma("pool", Wukv, w_ukv_d[layer], Wukv)
        P.dma("pool", Wout, w_out_d[layer].rearrange("(k p) n -> p k n", p=128), Wout)
        P.dma("pool", WcT, sguT_d[layer], WcT)
        P.dma("pool", sgn, sgn_d[layer], sgn)
        P.dma("pool", sgb, sgb_d[layer], sgb)
        P.dma("sp", qln, qln_d[layer], qln)
        P.dma("sp", kvln, kvln_d[layer], kvln)
        P.dma("sp", qn, qn_d[layer], qn)
        P.dma("sp", kn, kn_d[layer], kn)
        compute_mod(layer, 0, nmix_d)
        P.tt(WcT, WcT, tri.unsq(1).bc([128, 8, 128]), ALU.mult)
        P.tt(Wout, Wout, modb[:, 2, :].unsq(1).bc([128, 8, D]), ALU.mult)
        P.tt(GQ, qn[:, 0:128], kn[:, 0:128], ALU.mult)
        KTt = P.tile([128, 4, S], BF16, "KT")
        RTt = P.tile([64, S], BF16, "RT")
        Vt = P.tile([128, NT, 512], BF16, "V")
        rstdk = P.tile([128, NT, 4], F32, "rstdk")
        kbufs = [Buf(f"kblk{j}") for j in range(8)]
        hT = P.tile([128, 8, 512], BF16, "hT")
        QTn = P.tile([128, 4, 512], BF16, "QTn")
        QTr = P.tile([64, 4, 512], BF16, "QTr")
        catT = P.tile([128, 8, 512], BF16, "catT")
        xts = [P.tile([128, D], F32, f"xt{i}") for i in range(2)]
        junk = P.tile([128, D], F32, "junk")
        hbf = P.tile([128, D], BF16, "hbf")
        ss = P.tile([128, 1], F32, "ss")
        ms = P.tile([128, 1], F32, "ms")
        rstd = P.tile([128, 1], F32, "rstd")
        gu = P.tile([128, 512], BF16, "gu")
        gv = P.tile([128, 512], F32, "gv")
        st6 = P.tile([128, 6], F32, "st6")
        mv = P.tile([128, 2], F32, "mv")
        rv = P.tile([128, 1], F32, "rv")
        vtmp = P.tile([128, 512], F32, "vtmp")
        vb = P.tile([128, 512], BF16, "vb")
        stmp = vtmp
        ab = P.tile([128, 512], BF16, "ab")
        lat = P.tile([128, 448], F32, "lat")
        sl = P.tile([128, 4], F32, "sl")
        latn = P.tile([128, 384], BF16, "latn")
        latT = P.tile([128, 3, 128], BF16, "latT")
        qf = P.tile([128, 768], F32, "qf")
        sq = P.tile([128, 768], F32, "sq")
        ssq = P.tile([128, 4], F32, "ssq")
        rq = P.tile([128, 4], F32, "rq")
        qnb = P.tile([128, 4, 128], BF16, "qnb")
        qrb = P.tile([128, 4, 64], BF16, "qrb")
        r1 = P.tile([128, 4, 64], F32, "r1")
        r2 = P.tile([128, 4, 64], F32, "r2")
        r3 = P.tile([128, 4, 64], F32, "r3")
        knb = P.tile([128, 4, 128], BF16, "knb")
        ssk = P.tile([128, 4], F32, "ssk")
        sskr = P.tile([128, 1], F32, "sskr")
        krb = P.tile([128, 64], BF16, "krb")
        k1 = P.tile([128, 64], F32, "k1")
        k2 = P.tile([128, 64], F32, "k2")
        k3 = P.tile([128, 64], F32, "k3")
        Es = [P.tile([128, 512], BF16, f"E{i}") for i in range(2)]
        rec = gv
        xr = [P.tile([128, D], F32, f"xr{i}") for i in range(2)]
        ytmp = junk

        nblk = (nt_run + 3) // 4
        for j in range(nblk):
            kb = kbufs[j]
            def g_prelude(t, ti):
                X = (PS[1], PS[2], PS[3]) if ti % 2 == 0 else (PS[4], PS[5], PS[6])
                c0 = ti * 128
                xt = xts[t % 2]
                P.dma("sp", xt, src_d[t * 128:(t + 1) * 128, :], xt); yield
                P.act(junk, xt, AF.Square, accum=ss); yield
                P.ts(ms, ss, 1.0 / D, EPS, ALU.mult, ALU.add); yield
                P.rsqrt(rstd, ms, nhalf[:, 0:1]); yield
                P.stt(junk, xt, rstd, modb[:, 0, :], ALU.mult, ALU.mult); yield
                P.tt(hbf, junk, modb[:, 1, :], ALU.add); yield
                tp = X[0].bitcast(BF16)
                P.pe([("tr", tp[:, jj * 128:(jj + 1) * 128], hbf[:, jj * 128:(jj + 1) * 128], ident) for jj in range(8)]); yield
                P.copy(hT[:, :, c0:c0 + 128], tp.re("p (k t) -> p k t", k=8), eng="act"); yield
                P.pe([("mm", X[2][:, 0:448], hT[:, k, c0:c0 + 128], Win[:, k, 1024:1472], k == 0, k == 7) for k in range(8)]); yield
                P.pe([("mm", X[1], hT[:, k, c0:c0 + 128], Win[:, k, 512:1024], k == 0, k == 7) for k in range(8)]); yield
                P.pe([("mm", X[0], hT[:, k, c0:c0 + 128], Win[:, k, 0:512], k == 0, k == 7) for k in range(8)]); yield

            for ti in range(4):
                t = 4 * j + ti
                c0 = ti * 128
                X = (PS[1], PS[2], PS[3]) if ti % 2 == 0 else (PS[4], PS[5], PS[6])
                if ti == 0:
                    for _ in g_prelude(t, ti):
                        pass
                sqk = Es[0].bitcast(F32)
                state = {"lat": False}

                def g_sgu(c0=c0):
                    P.act(gu, X[0], AF.Gelu_apprx_tanh); yield
                    P.act(gv, X[1], AF.Gelu_apprx_tanh); yield
                    P.emit("dve", lambda e: e.bn_stats(out=st6.ap, in_=gv.ap), [gv.buf], [st6.buf]); yield
                    P.emit("dve", lambda e: e.bn_aggr(out=mv.ap, in_=st6.ap), [st6.buf], [mv.buf]); yield
                    P.ts(rv, mv[:, 1:2], EPS, None, ALU.add); yield
                    P.rsqrt(rv, rv, nhalf[:, 0:1]); yield
                    P.ts(vtmp, gv, mv[:, 0:1], rv, ALU.subtract, ALU.mult); yield
                    P.tt(vb, vtmp, sgn, ALU.mult); yield
                    P.pe([("mm", PS[0][:, g * 64:(g + 1) * 64], WcT[:, g, :], vb[:, g * 64:(g + 1) * 64], True, True)
                          for g in range(8)]); yield
                    P.tt(stmp, PS[0], sgb, ALU.add); yield
                    P.tt(ab, stmp, gu, ALU.mult); yield
                    tp = PS[0].bitcast(BF16)
                    P.pe([("tr", tp[:, g * 128:(g + 1) * 128], ab[:, g * 128:(g + 1) * 128], ident) for g in range(4)]); yield
                    P.copy(catT[:, 0:4, c0:c0 + 128], tp[:, 0:512].re("p (k t) -> p k t", k=4), eng="act"); yield

                def g_latq(c0=c0, t=t):
                    P.copy(lat, X[2][:, 0:448], eng="act"); yield
                    P.stt(sq[:, 0:256], lat[:, 0:256], 1.0, lat[:, 0:256], ALU.mult, ALU.mult, accum=sl[:, 0:1]); yield
                    P.stt(sq[:, 256:384], lat[:, 256:384], 1.0, lat[:, 256:384], ALU.mult, ALU.mult, accum=sl[:, 1:2]); yield
                    P.stt(sq[:, 384:448], lat[:, 384:448], 1.0, lat[:, 384:448], ALU.mult, ALU.mult, accum=sskr); yield
                    P.ts(sl[:, 2:3], sl[:, 0:1], 1.0 / 256, EPS, ALU.mult, ALU.add); yield
                    P.ts(sl[:, 3:4], sl[:, 1:2], 1.0 / 128, EPS, ALU.mult, ALU.add); yield
                    P.rsqrt(sl[:, 2:4], sl[:, 2:4], nhalf[:, 0:2]); yield
                    P.stt(latn[:, 0:256], lat[:, 0:256], sl[:, 2:3], qln, ALU.mult, ALU.mult); yield
                    P.stt(latn[:, 256:384], lat[:, 256:384], sl[:, 3:4], kvln, ALU.mult, ALU.mult); yield
                    tp = X[2].bitcast(BF16)
                    P.pe([("tr", tp[:, g * 128:(g + 1) * 128], latn[:, g * 128:(g + 1) * 128], ident) for g in range(3)]); yield
                    P.copy(latT, tp[:, 0:384].re("p (k t) -> p k t", k=3), eng="act"); yield
                    P.pe([("mm", X[2], latT[:, 2, :], Wukv[:, 0:512], True, True),
                          ("mm", PS[7], latT[:, 2, :], Wukv[:, 512:1024], True, True)])
                    state["lat"] = True
                    yield
                    P.pe([("mm", X[0], latT[:, 0, :], Wuq[:, 0, 0:512], True, False),
                          ("mm", X[0], latT[:, 1, :], Wuq[:, 1, 0:512], False, True),
                          ("mm", X[1][:, 0:256], latT[:, 0, :], Wuq[:, 0, 512:768], True, False),
                          ("mm", X[1][:, 0:256], latT[:, 1, :], Wuq[:, 1, 512:768], False, True)]); yield
                    P.copy(qf[:, 0:512], X[0], eng="act"); yield
                    P.copy(qf[:, 512:768], X[1][:, 0:256], eng="act"); yield
                    P.tt(sq, qf, qf, ALU.mult); yield
                    P.reduce(ssq, sq.re("p (h d) -> p h d", h=4)); yield
                    P.ts(ssq, ssq, 1.0 / 192, EPS, ALU.mult, ALU.add); yield
                    P.rsqrt(rq, ssq, nhalf[:, 0:4]); yield
                    q3 = qf.re("p (h d) -> p h d", h=4)
                    sq3 = sq.re("p (h d) -> p h d", h=4)
                    P.tt(sq3[:, :, 0:128], q3[:, :, 0:128], GQ.unsq(1).bc([128, 4, 128]), ALU.mult); yield
                    P.tt(qnb, sq3[:, :, 0:128], rq.unsq(2).bc([128, 4, 128]), ALU.mult); yield
                    tp = X[0].bitcast(BF16)
                    P.pe([("tr", tp[:, h * 128:(h + 1) * 128], qnb[:, h, :], ident) for h in range(4)]); yield
                    P.copy(QTn[:, :, c0:c0 + 128], tp[:, 0:512].re("p (k t) -> p k t", k=4), eng="act"); yield
                    cs = cosT[:, t, :].unsq(1).bc([128, 4, 32])
                    sn = sinT[:, t, :].unsq(1).bc([128, 4, 32])
                    P.tt(r1, q3[:, :, 128:192], qn[:, 128:192].unsq(1).bc([128, 4, 64]), ALU.mult); yield
                    P.tt(r2[:, :, 0:32], r1[:, :, 0:32], cs, ALU.mult); yield
                    P.tt(r2[:, :, 32:64], r1[:, :, 32:64], cs, ALU.mult); yield
                    P.tt(r3[:, :, 0:32], r1[:, :, 32:64], sn, ALU.mult); yield
                    P.tt(r3[:, :, 32:64], r1[:, :, 0:32], sn, ALU.mult); yield
                    P.tt(r2[:, :, 0:32], r2[:, :, 0:32], r3[:, :, 0:32], ALU.subtract); yield
                    P.tt(r2[:, :, 32:64], r2[:, :, 32:64], r3[:, :, 32:64], ALU.add); yield
                    P.tt(qrb, r2, rq.unsq(2).bc([128, 4, 64]), ALU.mult); yield
                    tp = X[1].bitcast(BF16)
                    P.pe([("tr", tp[0:64, h * 128:(h + 1) * 128], qrb[:, h, :], ident) for h in range(4)]); yield
                    P.copy(QTr[:, :, c0:c0 + 128], tp[0:64, 0:512].re("p (k t) -> p k t", k=4), eng="act"); yield

                def g_kv(t=t, kb=kb):
                    while not state["lat"]:
                        yield
                    for half in range(2):
                        kv3 = (X[2] if half == 0 else PS[7]).re("p (h c) -> p h c", h=2)
                        P.copy(Vt[:, t, half * 256:(half + 1) * 256].re("p (h d) -> p h d", h=2).on(kb),
                               kv3[:, :, 128:256], eng="act"); yield
                        P.copy(knb[:, 2 * half:2 * half + 2, :], kv3[:, :, 0:128], eng="act"); yield
                        P.tt(sqk.re("p (h d) -> p h d", h=2), kv3[:, :, 0:128], knb[:, 2 * half:2 * half + 2, :],
                             ALU.mult); yield
                        P.reduce(ssk[:, 2 * half:2 * half + 2], sqk.re("p (h d) -> p h d", h=2)); yield
                    P.ts(ssk, ssk, sskr, 1.0 / 192, ALU.add, ALU.mult); yield
                    P.ts(ssk, ssk, EPS, None, ALU.add); yield
                    P.rsqrt(ssk, ssk, nhalf[:, 0:4]); yield
                    P.ts(rstdk[:, t, :].on(kb), ssk, float(192 ** -0.5), None, ALU.mult); yield
                    tp = PS[7].bitcast(BF16)
                    P.pe([("tr", tp[:, h * 128:(h + 1) * 128], knb[:, h, :], ident) for h in range(4)]); yield
                    P.copy(KTt[:, :, t * 128:(t + 1) * 128].on(kb), tp[:, 0:512].re("p (k t) -> p k t", k=4), eng="act"); yield
                    cs1 = cosT[:, t, :]
                    sn1 = sinT[:, t, :]
                    P.tt(k1, lat[:, 384:448], kn[:, 128:192], ALU.mult, eng="pool"); yield
                    P.tt(k2[:, 0:32], k1[:, 0:32], cs1, ALU.mult, eng="pool"); yield
                    P.tt(k2[:, 32:64], k1[:, 32:64], cs1, ALU.mult, eng="pool"); yield
                    P.tt(k3[:, 0:32], k1[:, 32:64], sn1, ALU.mult, eng="pool"); yield
                    P.tt(k3[:, 32:64], k1[:, 0:32], sn1, ALU.mult, eng="pool"); yield
                    P.tt(krb[:, 0:32], k2[:, 0:32], k3[:, 0:32], ALU.subtract, eng="pool"); yield
                    P.tt(krb[:, 32:64], k2[:, 32:64], k3[:, 32:64], ALU.add, eng="pool"); yield
                    tp = PS[7].bitcast(BF16)
                    P.pe([("tr", tp[0:64, 0:128], krb, ident)]); yield
                    P.copy(RTt[:, t * 128:(t + 1) * 128].on(kb), tp[0:64, 0:128], eng="act"); yield

                gens = [g_sgu(), g_latq(), g_kv()]
                if ti < 3:
                    gens.insert(0, g_prelude(t + 1, ti + 1))
                while gens:
                    for g in list(gens):
                        try:
                            next(g)
                        except StopIteration:
                            gens.remove(g)

            nkt = 4 * (j + 1)
            units = [(h, kt) for h in range(4) for kt in range(nkt)]
            Sb = [PS[0], PS[1]]

            def emit_S(i):
                h, kt = units[i]
                kbk = kbufs[kt // 4]
                qoff = max(0, kt - 4 * j) * 128
                sb = Sb[i % 2]
                P.pe([("mm", sb[:, qoff:512], KTt[:, h, kt * 128:(kt + 1) * 128].on(kbk), QTn[:, h, qoff:512], True, False),
                      ("mm", sb[:, qoff:512], RTt[:, kt * 128:(kt + 1) * 128].on(kbk), QTr[:, h, qoff:512], False, True)])

            emit_S(0)
            for i, (h, kt) in enumerate(units):
                if i + 1 < len(units):
                    emit_S(i + 1)
                kbk = kbufs[kt // 4]
                qoff = max(0, kt - 4 * j) * 128
                sb = Sb[i % 2]
                Ob = PS[2 + (h % 2)]
                Lb = PS[4 + (h % 2)]
                E = Es[i % 2]
                P.act(E[:, qoff:512], sb[:, qoff:512], AF.Exp, scale=rstdk[:, kt, h:h + 1].on(kbk))
                if kt >= 4 * j:
                    P.tt(E[:, qoff:qoff + 128], E[:, qoff:qoff + 128], tri, ALU.mult, eng="pool")
                P.pe([("mm", Ob[:, qoff:512], Vt[:, kt, h * 128:(h + 1) * 128].on(kbk), E[:, qoff:512], kt == 0, kt == nkt - 1),
                      ("mm", Lb[:, qoff:512], ones, E[:, qoff:512], kt == 0, kt == nkt - 1)])
                if kt == nkt - 1:
                    P.recip(rec, Lb)
                    P.tt(catT[:, 4 + h, :], Ob, rec, ALU.mult)

            P.dma("sp", xr[(4 * j) % 2], src_d[4 * j * 128:(4 * j + 1) * 128, :], xr[(4 * j) % 2])
            for ti in range(4):
                t = 4 * j + ti
                c0 = ti * 128
                x2 = xr[t % 2]
                if ti < 3:
                    P.dma("sp", xr[(t + 1) % 2], src_d[(t + 1) * 128:(t + 2) * 128, :], xr[(t + 1) % 2])
                items = []
                pa, pb = [(PS[6], PS[7]), (PS[0], PS[1]), (PS[2], PS[3]), (PS[4], PS[5])][ti]
                for k in range(8):
                    l = catT[:, k, c0:c0 + 128]
                    items.append(("mm", pa, l, Wout[:, k, 0:512], k == 0, k == 7))
                    items.append(("mm", pb, l, Wout[:, k, 512:1024], k == 0, k == 7))
                P.pe(items)
                o = x2
                P.tt(o[:, 0:512], pa, x2[:, 0:512], ALU.add)
                P.tt(o[:, 512:1024], pb, x2[:, 512:1024], ALU.add)
                P.dma("sp", dst_d[t * 128:(t + 1) * 128, :], o, o)
        P.barrier()
        P.sb_off = m

    def ffn(layer, src_d, dst_d, w1s, w3s, w2s, moe):
        m = P.sb_off
        compute_mod(layer, 1, nffn_d)
        nexp = len(w1s)
        TB = 1024
        hTs = [P.tile([128, 8, TB], BF16, f"fhT{i}") for i in range(2 if not moe else 1)]
        actT = P.tile([128, NF, TB], BF16, "actT")
        W2 = P.tile([128, NF, D], BF16, "W2")
        NWB = 4
        W1g = [P.tile([128, 8, 256], BF16, f"W1g{i}") for i in range(NWB)]
        W3g = [P.tile([128, 8, 256], BF16, f"W3g{i}") for i in range(NWB)]
        xts = [P.tile([128, D], F32, f"fx{i}") for i in range(2)]
        junk = P.tile([128, D], F32, "fjunk")
        hbf = P.tile([128, D], BF16, "fhbf")
        ss = P.tile([128, 1], F32, "fss")
        ms = P.tile([128, 1], F32, "fms")
        rstd = P.tile([128, 1], F32, "frstd")
        sg = [P.tile([128, 512], BF16, f"sg{i}") for i in range(2)]
        xr = [P.tile([128, D], F32, f"fxr{i}") for i in range(2)]
        ytmp = junk
        if moe:
            acc = P.tile([128, 8, D], F32, "acc")
            h32 = P.tile([128, D], F32, "h32")
            hT32 = P.tile([128, 8, 128], F32, "hT32")
            rw = P.tile([128, 8, NE], F32, "rw")
            lg = P.tile([128, NE], F32, "lg")
            lg2 = P.tile([128, NE], F32, "lg2")
            mk1 = P.tile([128, NE], F32, "mk1")
            mk2 = P.tile([128, NE], F32, "mk2")
            m1 = P.tile([128, 1], F32, "m1")
            m2 = P.tile([128, 1], F32, "m2")
            dd = P.tile([128, 1], F32, "dd")
            w1g = P.tile([128, 1], F32, "w1g")
            w2g = P.tile([128, 1], F32, "w2g")
            gates = P.tile([128, 8, NE], F32, "gates")
            P.dma("sp", rw, rw_d[0].rearrange("(k p) e -> p k e", p=128), rw)
        nblk = max(1, nt_run // 8)
        gi = 0
        hbf2 = [hbf, P.tile([128, D], BF16, "fhbf2")]

        def norm_a(blk, ti):
            t = blk * 8 + ti
            xt = xts[t % 2]
            hb = hbf2[ti % 2]
            P.dma("sp", xt, src_d[t * 128:(t + 1) * 128, :], xt)
            P.act(junk, xt, AF.Square, accum=ss)
            P.ts(ms, ss, 1.0 / D, EPS, ALU.mult, ALU.add)
            P.rsqrt(rstd, ms, nhalf[:, 0:1])
            P.stt(junk, xt, rstd, modb[:, 0, :], ALU.mult, ALU.mult)
            P.tt(hb, junk, modb[:, 1, :], ALU.add)

        def norm_b(blk, ti, bank):
            hb = hbf2[ti % 2]
            tp = PS[bank].bitcast(BF16)
            P.pe([("tr", tp[:, j * 128:(j + 1) * 128], hb[:, j * 128:(j + 1) * 128], ident) for j in range(8)])
            P.copy(hTs[blk % len(hTs)][:, :, ti * 128:(ti + 1) * 128], tp.re("p (k t) -> p k t", k=8), eng="act")

        def load_chunk(ci):
            if ci >= nblk * (NF // 2):
                return
            fgc = ci % (NF // 2)
            wa_, wb_ = W1g[ci % NWB], W3g[ci % NWB]
            P.dma("pool", wa_.re("p k c -> p (k c)"), w1s[0][fgc * 128:(fgc + 1) * 128, :], wa_)
            P.dma("pool", wb_.re("p k c -> p (k c)"), w3s[0][fgc * 128:(fgc + 1) * 128, :], wb_)

        if not moe:
            for ti in range(8):
                norm_a(0, ti)
                if ti >= 1:
                    norm_b(0, ti - 1, 0)
            norm_b(0, 7, 0)
        for blk in range(nblk):
            hT = hTs[blk % len(hTs)]
            for ti in range(8 if moe else 0):
                t = blk * 8 + ti
                xt = xts[t % 2]
                P.dma("sp", xt, src_d[t * 128:(t + 1) * 128, :], xt)
                norm_tile(xt, hT[:, :, ti * 128:(ti + 1) * 128], (junk, ss, ms, rstd, hbf, PS[0]),
                          h32=h32 if moe else None)
                if moe:
                    for hh in range(2):
                        P.pe([("tr", PS[1 + hh][:, g * 128:(g + 1) * 128],
                               h32[:, (hh * 4 + g) * 128:(hh * 4 + g + 1) * 128], ident32) for g in range(4)])
                        P.copy(hT32[:, hh * 4:hh * 4 + 4, :], PS[1 + hh].re("p (k t) -> p k t", k=4), eng="act")
                    P.pe([("mm", PS[3][:, 0:NE], hT32[:, k, :], rw[:, k, :], k == 0, k == 7) for k in range(8)])
                    P.copy(lg, PS[3][:, 0:NE])
                    P.reduce(m1, lg, op=ALU.max)
                    P.ts(mk1, lg, m1, None, ALU.is_ge)
                    P.stt(lg2, mk1, -1e30, lg, ALU.mult, ALU.add)
                    P.reduce(m2, lg2, op=ALU.max)
                    P.ts(mk2, lg2, m2, None, ALU.is_ge)
                    P.tt(dd, m2, m1, ALU.subtract)
                    P.act(dd, dd, AF.Exp)
                    P.ts(dd, dd, 1.0, None, ALU.add)
                    P.recip(w1g, dd)
                    P.ts(w2g, w1g, -1.0, 1.0, ALU.mult, ALU.add)
                    P.ts(mk1, mk1, w1g, None, ALU.mult)
                    P.stt(gates[:, ti, :], mk2, w2g, mk1, ALU.mult, ALU.add)
            for e in range(nexp):
                w1d, w3d, w2d = w1s[e], w3s[e], w2s[e]
                if moe:
                    P.dma("pool", W2, w2d.rearrange("(f p) n -> p f n", p=128), W2)
                else:
                    P.dma("pool", W2.re("p f n -> p (f n)"), w2d, W2)
                if not moe:
                    P.tt(W2, W2, modb[:, 2, :].unsq(1).bc([128, NF, D]), ALU.mult)
                for fg in range(NF // 2):
                    wa, wb = W1g[gi % NWB], W3g[gi % NWB]
                    if moe:
                        P.dma("pool", wa, w1d[:, fg * 256:(fg + 1) * 256].rearrange("(k p) n -> p k n", p=128), wa)
                        P.dma("pool", wb, w3d[:, fg * 256:(fg + 1) * 256].rearrange("(k p) n -> p k n", p=128), wb)
                    else:
                        if gi == 0:
                            for pj in range(NWB - 1):
                                load_chunk(pj)
                        load_chunk(gi + NWB - 1)
                    gi += 1
                    for fi in range(2):
                        f = fg * 2 + fi
                        for sub in range(2):
                            gp = PS[(2 * sub) % 4]
                            up = PS[(2 * sub + 1) % 4]
                            tok = slice(sub * 512, (sub + 1) * 512)
                            P.pe([("mm", gp, wa[:, k, fi * 128:(fi + 1) * 128], hT[:, k, tok], k == 0, k == 7)
                                  for k in range(8)])
                            P.pe([("mm", up, wb[:, k, fi * 128:(fi + 1) * 128], hT[:, k, tok], k == 0, k == 7)
                                  for k in range(8)])
                            s_ = sg[sub]
                            P.act(s_, gp, AF.Silu)
                            P.tt(actT[:, f, tok], s_, up, ALU.mult)
                    if (not moe) and blk + 1 < nblk:
                        if 1 <= fg <= 8:
                            norm_b(blk + 1, fg - 1, 7)
                        if fg < 8:
                            norm_a(blk + 1, fg)
                for ti in range(8):
                    t = blk * 8 + ti
                    c0 = ti * 128
                    pa, pb = PS[4 + 2 * (ti % 2)], PS[5 + 2 * (ti % 2)]
                    items = []
                    for f in range(NF):
                        l = actT[:, f, c0:c0 + 128]
                        items.append(("mm", pa, l, W2[:, f, 0:512], f == 0, f == NF - 1))
                        items.append(("mm", pb, l, W2[:, f, 512:1024], f == 0, f == NF - 1))
                    P.pe(items)
                    if moe:
                        gcol = gates[:, ti, e:e + 1]
                        if e == 0:
                            P.ts(acc[:, ti, 0:512], pa, gcol, None, ALU.mult)
                            P.ts(acc[:, ti, 512:1024], pb, gcol, None, ALU.mult)
                        else:
                            P.stt(acc[:, ti, 0:512], pa, gcol, acc[:, ti, 0:512], ALU.mult, ALU.add)
                            P.stt(acc[:, ti, 512:1024], pb, gcol, acc[:, ti, 512:1024], ALU.mult, ALU.add)
                    if (not moe) or e == nexp - 1:
                        x2 = xr[t % 2]
                        if ti == 0:
                            P.dma("sp", x2, src_d[t * 128:(t + 1) * 128, :], x2)
                        if ti < 7:
                            P.dma("sp", xr[(t + 1) % 2], src_d[(t + 1) * 128:(t + 2) * 128, :], xr[(t + 1) % 2])
                        o = x2
                        if moe:
                            P.tt(ytmp, acc[:, ti, :], modb[:, 2, :], ALU.mult, eng="pool")
                            P.tt(o, ytmp, x2, ALU.add, eng="pool")
                        else:
                            P.tt(o[:, 0:512], pa, x2[:, 0:512], ALU.add)
                            P.tt(o[:, 512:1024], pb, x2[:, 512:1024], ALU.add)
                        P.dma("sp", dst_d[t * 128:(t + 1) * 128, :], o, o)
        P.barrier()
        P.sb_off = m

    def moe_sparse(layer, src_d, dst_d):
        m = P.sb_off
        compute_mod(layer, 1, nffn_d)
        TB = 1024
        xts = [P.tile([128, D], F32, f"sx{i}") for i in range(2)]
        hbfs = [P.tile([128, D], BF16, f"shbf{i}") for i in range(2)]
        ysb = [P.tile([128, D], F32, f"sys{i}") for i in range(4)]
        gw = P.tile([128, NT, 2], F32, "sgw")
        ridx = P.tile([128, NT, 2], I32, "sridx")
        iH = P.tile([128, 16, 8], I32, "siH")
        iW = P.tile([128, 16, 11], I32, "siW")
        iW2 = P.tile([128, 16, 2], I32, "siW2")
        mark = P.sb_off
        rw = P.tile([128, 8, NE], F32, "srw")
        runc = P.tile([128, NE], F32, "srunc")
        ebase = P.tile([128, NE], F32, "sebase")
        ustr = P.tile([128, 128], BF16, "sustr")
        giota = P.tile([128, 16], F32, "sgiota")
        cth = P.tile([128, 8], F32, "scth")
        ctw = P.tile([128, 11], F32, "sctw")
        ctw2 = P.tile([128, 2], F32, "sctw2")
        ng = P.tile([128, NE], F32, "sng")
        cum = P.tile([128, NE], F32, "scum")
        tmp8 = P.tile([128, NE], F32, "stmp8")
        E16 = P.tile([128, 16], F32, "sE16")
        O16 = P.tile([128, 16], F32, "sO16")
        t16 = P.tile([128, 16], F32, "st16")
        B16 = P.tile([128, 16], F32, "sB16")
        fH = P.tile([128, 16, 8], F32, "sfH")
        fW = P.tile([128, 16, 11], F32, "sfW")
        fW2 = P.tile([128, 16, 2], F32, "sfW2")

        def s1_set(i):
            d = {}
            d["junk"] = P.tile([128, D], F32, f"sjunk{i}")
            d["h32"] = P.tile([128, D], F32, f"sh32{i}")
            d["hT32"] = P.tile([128, 8, 128], F32, f"shT32{i}")
            for nm in ("ss", "ms", "rstd", "m1", "m2", "dd"):
                d[nm] = P.tile([128, 1], F32, f"s{nm}{i}")
            for nm in ("lg", "lg2", "mk1", "mk2", "rk", "rk2"):
                d[nm] = P.tile([128, NE], F32, f"s{nm}{i}")
            d["mk12"] = P.tile([128, NE], BF16, f"smk12{i}")
            d["rf"] = P.tile([128, 2], F32, f"srf{i}")
            d["banks"] = (PS[1], PS[2], PS[3], PS[4]) if i == 0 else (PS[5], PS[6], PS[7], PS[0])
            return d
        sets = [s1_set(0), s1_set(1)]
        P.dma("sp", rw, rw_d[0].rearrange("(k p) e -> p k e", p=128), rw)
        P.dma("sp", ebase, ebase_d, ebase)
        P.dma("sp", giota, giota_d, giota)
        P.dma("sp", cth, cth_d, cth)
        P.dma("sp", ctw, ctw_d, ctw)
        P.dma("sp", ctw2, ctw2_d, ctw2)
        P.tt(ustr, tri, ident, ALU.subtract)
        P.memset(runc, 0.0)

        def g_route(t):
            d = sets[t % 2]
            junk, h32, hT32 = d["junk"], d["h32"], d["hT32"]
            ss, ms, rstd, m1, m2, dd = d["ss"], d["ms"], d["rstd"], d["m1"], d["m2"], d["dd"]
            lg, lg2, mk1, mk2, rk, rk2, mk12, rf = d["lg"], d["lg2"], d["mk1"], d["mk2"], d["rk"], d["rk2"], d["mk12"], d["rf"]
            bt0, bt1, blg, brk = d["banks"]
            xt = xts[t % 2]
            hbf = hbfs[t % 2]
            P.dma("sp", xt, src_d[t * 128:(t + 1) * 128, :], xt); yield
            P.act(junk, xt, AF.Square, accum=ss); yield
            P.ts(ms, ss, 1.0 / D, EPS, ALU.mult, ALU.add); yield
            P.rsqrt(rstd, ms, nhalf[:, 0:1]); yield
            P.stt(h32, xt, rstd, modb[:, 0, :], ALU.mult, ALU.mult); yield
            P.tt(h32, h32, modb[:, 1, :], ALU.add); yield
            P.copy(hbf, h32, eng="act"); yield
            for hh, bk in ((0, bt0), (1, bt1)):
                P.pe([("tr", bk[:, g * 128:(g + 1) * 128],
                       h32[:, (hh * 4 + g) * 128:(hh * 4 + g + 1) * 128], ident32) for g in range(4)]); yield
                P.copy(hT32[:, hh * 4:hh * 4 + 4, :], bk.re("p (k t) -> p k t", k=4), eng="act"); yield
            P.pe([("mm", blg[:, 0:NE], hT32[:, k, :], rw[:, k, :], k == 0, k == 7) for k in range(8)]); yield
            P.copy(lg, blg[:, 0:NE]); yield
            P.reduce(m1, lg, op=ALU.max); yield
            P.ts(mk1, lg, m1, None, ALU.is_ge); yield
            P.stt(lg2, mk1, -1e30, lg, ALU.mult, ALU.add); yield
            P.reduce(m2, lg2, op=ALU.max); yield
            P.ts(mk2, lg2, m2, None, ALU.is_ge); yield
            P.tt(dd, m2, m1, ALU.subtract); yield
            P.act(dd, dd, AF.Exp); yield
            P.ts(dd, dd, 1.0, None, ALU.add); yield
            P.recip(gw[:, t, 0:1], dd); yield
            P.ts(gw[:, t, 1:2], gw[:, t, 0:1], -1.0, 1.0, ALU.mult, ALU.add); yield
            P.tt(mk12, mk1, mk2, ALU.add); yield
            P.pe([("mm", brk[:, 0:NE], ustr, mk12, True, True),
                  ("mm", brk[:, NE:2 * NE], ones, mk12, True, True)]); yield
            P.tt(rk, brk[:, 0:NE], runc, ALU.add)
            P.tt(runc, runc, brk[:, NE:2 * NE], ALU.add); yield
            P.tt(rk, rk, ebase, ALU.add); yield
            P.stt(rk2, rk, 1.0, mk1, ALU.mult, ALU.mult, accum=rf[:, 0:1]); yield
            P.stt(rk2, rk, 1.0, mk2, ALU.mult, ALU.mult, accum=rf[:, 1:2]); yield
            P.copy(ridx[:, t, :], rf); yield
            P.idma(hs_d, hbf, ridx[:, t, 0:1], hbf, True); yield
            P.idma(hs_d, hbf, ridx[:, t, 1:2], hbf, True); yield

        pend = []
        for t in range(nt_run):
            pend.append(g_route(t))
            for _ in range(20):
                for g in list(pend):
                    try:
                        next(g)
                    except StopIteration:
                        pend.remove(g)
        while pend:
            for g in list(pend):
                try:
                    next(g)
                except StopIteration:
                    pend.remove(g)

        P.memset(ng, 0.0)
        for jj in range(4):
            P.ts(tmp8, runc, float(jj * 1024) + 0.5, None, ALU.is_gt)
            P.tt(ng, ng, tmp8, ALU.add)
        P.copy(cum, ng)
        for e in range(1, NE):
            P.tt(cum[:, e:e + 1], cum[:, e:e + 1], cum[:, e - 1:e], ALU.add)
        P.memset(E16, 0.0)
        P.copy(O16, giota)
        for e in range(NE):
            P.ts(t16, giota, cum[:, e:e + 1], None, ALU.is_ge)
            P.tt(E16, E16, t16, ALU.add)
            P.ts(t16, t16, ng[:, e:e + 1], None, ALU.mult)
            P.tt(O16, O16, t16, ALU.subtract)
        P.ts(E16, E16, float(NE - 1), None, ALU.min)
        P.ts(O16, O16, 3.0, 0.0, ALU.min, ALU.max)
        P.ts(B16, E16, float(S), None, ALU.mult)
        P.stt(B16, O16, 1024.0, B16, ALU.mult, ALU.add)
        P.tt(fH, B16.unsq(2).bc([128, 16, 8]), cth.unsq(1).bc([128, 16, 8]), ALU.add)
        P.copy(iH, fH)
        P.ts(t16, E16, float(11 * 128), None, ALU.mult)
        P.tt(fW, t16.unsq(2).bc([128, 16, 11]), ctw.unsq(1).bc([128, 16, 11]), ALU.add)
        P.copy(iW, fW)
        P.ts(t16, E16, 256.0, None, ALU.mult)
        P.tt(fW2, t16.unsq(2).bc([128, 16, 2]), ctw2.unsq(1).bc([128, 16, 2]), ALU.add)
        P.copy(iW2, fW2)
        P.barrier()

        P.sb_off = mark
        hTs = [P.tile([128, 8, TB], BF16, f"shT{i}") for i in range(2)]
        actT = P.tile([128, NF, TB], BF16, "sactT")
        W2 = P.tile([128, NF, D], BF16, "sW2")
        NWB = 4
        W1g = [P.tile([128, 8, 256], BF16, f"sW1g{i}") for i in range(NWB)]
        W3g = [P.tile([128, 8, 256], BF16, f"sW3g{i}") for i in range(NWB)]
        sg = [P.tile([128, 512], BF16, f"ssg{i}") for i in range(2)]
        ngroups = 16 if n_tiles_dbg is None else max(1, n_tiles_dbg // 2)
        gi = 0
        W2f = W2.re("p f n -> p (f n)")

        def slot_a(g, ti):
            hsl = hbfs[ti % 2]
            P.idma(hsl, hs_d, iH[:, g, ti:ti + 1], hsl, False)

        def slot_b(g, ti, bank):
            hsl = hbfs[ti % 2]
            tp = PS[bank].bitcast(BF16)
            P.pe([("tr", tp[:, j * 128:(j + 1) * 128], hsl[:, j * 128:(j + 1) * 128], ident) for j in range(8)])
            P.copy(hTs[g % 2][:, :, ti * 128:(ti + 1) * 128], tp.re("p (k t) -> p k t", k=8), eng="act")

        for g in range(ngroups):
            for half in range(2):
                P.idma(W2f[:, half * 11 * D:(half + 1) * 11 * D], w2L_d, iW2[:, g, half:half + 1], W2, False)
            hT = hTs[g % 2]
            if g == 0:
                for ti in range(8):
                    slot_a(0, ti)
                    slot_b(0, ti, ti % 2)
            for fg in range(NF // 2):
                wa, wb = W1g[gi % NWB], W3g[gi % NWB]
                gi += 1
                P.idma(wa.re("p k c -> p (k c)"), w1L_d, iW[:, g, fg:fg + 1], wa, False)
                P.idma(wb.re("p k c -> p (k c)"), w3L_d, iW[:, g, fg:fg + 1], wb, False)
                for fi in range(2):
                    f = fg * 2 + fi
                    for sub in range(2):
                        gp = PS[(2 * sub) % 4]
                        up = PS[(2 * sub + 1) % 4]
                        tok = slice(sub * 512, (sub + 1) * 512)
                        P.pe([("mm", gp, wa[:, k, fi * 128:(fi + 1) * 128], hT[:, k, tok], k == 0, k == 7)
                              for k in range(8)])
                        P.pe([("mm", up, wb[:, k, fi * 128:(fi + 1) * 128], hT[:, k, tok], k == 0, k == 7)
                              for k in range(8)])
                        s_ = sg[sub]
                        P.act(s_, gp, AF.Silu)
                        P.tt(actT[:, f, tok], s_, up, ALU.mult)
                if g + 1 < ngroups:
                    if 1 <= fg <= 8:
                        slot_b(g + 1, fg - 1, 7)
                    if fg < 8:
                        slot_a(g + 1, fg)
            for ti in range(8):
                c0 = ti * 128
                pa, pb = PS[4 + 2 * (ti % 2)], PS[5 + 2 * (ti % 2)]
                items = []
                for f in range(NF):
                    l = actT[:, f, c0:c0 + 128]
                    items.append(("mm", pa, l, W2[:, f, 0:512], f == 0, f == NF - 1))
                    items.append(("mm", pb, l, W2[:, f, 512:1024], f == 0, f == NF - 1))
                P.pe(items)
                yb = ysb[ti % 2]
                P.copy(yb[:, 0:512], pa, eng="act")
                P.copy(yb[:, 512:1024], pb)
                P.idma(ys_d, yb, iH[:, g, ti:ti + 1], yb, True)
        P.barrier()

        def s4_loads(t):
            xt = xts[t % 2]
            y0, y1 = ysb[2 * (t % 2)], ysb[2 * (t % 2) + 1]
            P.dma("sp", xt, src_d[t * 128:(t + 1) * 128, :], xt)
            P.idma(y0, ys_d, ridx[:, t, 0:1], y0, False)
            P.idma(y1, ys_d, ridx[:, t, 1:2], y1, False)

        s4_loads(0)
        for t in range(nt_run):
            xt = xts[t % 2]
            y0, y1 = ysb[2 * (t % 2)], ysb[2 * (t % 2) + 1]
            if t + 1 < nt_run:
                s4_loads(t + 1)
            P.ts(y0, y0, gw[:, t, 0:1], None, ALU.mult)
            P.stt(y0, y1, gw[:, t, 1:2], y0, ALU.mult, ALU.add)
            P.tt(y0, y0, modb[:, 2, :], ALU.mult)
            P.tt(xt, y0, xt, ALU.add)
            P.dma("sp", dst_d[t * 128:(t + 1) * 128, :], xt, xt)
        P.barrier()
        P.sb_off = m

    phases = [
        ("mix0", lambda s, d: mixer(0, s, d)),
        ("ffn0", lambda s, d: ffn(0, s, d, [fw1L_d], [fw3L_d], [fw2L_d], False)),
        ("mix1", lambda s, d: mixer(1, s, d)),
        ("ffn1", (lambda s, d: moe_sparse(1, s, d)) if SPARSE else
         (lambda s, d: ffn(1, s, d, [moe_w1_d[0, e] for e in range(NE)],
                           [moe_w3_d[0, e] for e in range(NE)],
                           [moe_w2_d[0, e] for e in range(NE)], True))),
    ]
    if stop_after == "ffn1only":
        phases = phases[3:]
    elif stop_after is not None:
        phases = phases[:[p[0] for p in phases].index(stop_after) + 1]
    for i, (name, fn) in enumerate(phases):
        src = x_d if i == 0 else xs_d
        dst = y_d if i == len(phases) - 1 else xs_d
        fn(src, dst)
    P.barrier()
    P.finish()
    return nc, stack


def make_in_maps(inputs, cores):
    f = np.float32
    A = lambda a: np.ascontiguousarray(a)
    bc = lambda a: A(np.broadcast_to(a[:, None, :], (a.shape[0], 128, a.shape[1])))
    inv_freq = (1.0 / (np.float32(10000.0) ** (np.arange(0, 64, 2, dtype=f) / f(64)))).astype(f)
    shared = {
        "invf": A(np.broadcast_to(inv_freq[None, :], (128, 32))),
        "ident": np.eye(128, dtype=f),
        "tri": A(np.triu(np.ones((128, 128), dtype=f))),
        "ada_w": A(inputs["ada_w"]), "ada_b": A(inputs["ada_b"]),
        "nmix_b": bc(inputs["norm_mix"]), "nffn_b": bc(inputs["norm_ffn"]),
        "w_in": A(inputs["w_in"]),
        "sgn_b": bc(inputs["sgu_norm"]),
        "sguT": A(np.transpose(inputs["sgu_w"], (0, 3, 1, 2))),
        "sgb": A(np.repeat(np.transpose(inputs["sgu_b"], (0, 2, 1)), 64, axis=2)),
        "qln_b": bc(inputs["q_lat_norm"]), "kvln_b": bc(inputs["kv_lat_norm"]),
        "w_uq": A(inputs["w_uq"]), "w_ukv": A(inputs["w_ukv"]),
        "qn_b": bc(inputs["q_norm"]), "kn_b": bc(inputs["k_norm"]),
        "w_out": A(inputs["w_out"]),
        "fw1L": A(inputs["ffn_w1"][0].reshape(8, 128, 11, 256).transpose(2, 1, 0, 3).reshape(11 * 128, 2048)),
        "fw3L": A(inputs["ffn_w3"][0].reshape(8, 128, 11, 256).transpose(2, 1, 0, 3).reshape(11 * 128, 2048)),
        "fw2L": A(inputs["ffn_w2"][0].reshape(NF, 128, D).transpose(1, 0, 2).reshape(128, NF * D)),
        "router_w": A(inputs["router_w"]),
    }
    def lay13(w):
        return A(w[0].reshape(NE, 8, 128, 11, 256).transpose(0, 3, 2, 1, 4).reshape(NE * 11 * 128, 8 * 256))
    shared["w1L"] = lay13(inputs["moe_w1"])
    shared["w3L"] = lay13(inputs["moe_w3"])
    shared["w2L"] = A(inputs["moe_w2"][0].reshape(NE, 2, 11, 128, D).transpose(0, 3, 1, 2, 4).reshape(NE * 128 * 2, 11 * D))
    pp = np.arange(128, dtype=f)[:, None]
    shared["ebase"] = A(np.broadcast_to((np.arange(NE, dtype=f) * S)[None, :], (128, NE)))
    shared["giota"] = A(np.broadcast_to(np.arange(16, dtype=f)[None, :], (128, 16)))
    shared["ct_h"] = A(np.arange(8, dtype=f)[None, :] * 128 + pp)
    shared["ct_w"] = A(np.arange(11, dtype=f)[None, :] * 128 + pp)
    shared["ct_w2"] = A(np.arange(2, dtype=f)[None, :] + 2 * pp)
    maps = []
    for b in cores:
        mp = dict(shared)
        mp["x"] = A(inputs["x"][b])
        mp["cT"] = A(inputs["c"][b].reshape(8, 128).T)
        mp["pos"] = A(inputs["positions"][b].reshape(NT, 128).T.astype(np.int32))
        maps.append(mp)
    return maps


def kernel(**inputs):
    inputs = {k: np.asarray(v) for k, v in inputs.items()}
    nc, stack = build_program()
    with stack:
        in_maps = make_in_maps(inputs, list(range(8)))
        res = run_bass_kernel_spmd(nc, in_maps, core_ids=list(range(8)))
    return np.stack([np.asarray(r["y"], dtype=np.float32) for r in res.results], axis=0)
```

```python
import contextlib
import numpy as np
import concourse.bass as bass
import concourse.mybir as mybir
from concourse.bass_utils import run_bass_kernel_spmd

F32, BF16, I32 = mybir.dt.float32, mybir.dt.bfloat16, mybir.dt.int32
AF = mybir.ActivationFunctionType
ALU = mybir.AluOpType
AX = mybir.AxisListType

S = 4096
D = 1024
NT = S // 128
DFF = 2816
NF = DFF // 128
NE = 8
EPS = 1e-6
D_IN = 1472
ENGS = ("sp", "act", "dve", "pool", "pe")
_DT_SIZE = {F32: 4, BF16: 2, I32: 4}


class Buf:
    __slots__ = ("name", "w", "r", "dsem", "dcnt")

    def __init__(self, name):
        self.name = name
        self.w = None
        self.r = {}
        self.dsem = None
        self.dcnt = 0


class View:
    def __init__(self, ap, buf):
        self.ap = ap
        self.buf = buf

    def __getitem__(self, k):
        return View(self.ap[k], self.buf)

    def re(self, s, **kw):
        return View(self.ap.rearrange(s, **kw), self.buf)

    def bc(self, shape):
        return View(self.ap.to_broadcast(list(shape)), self.buf)

    def bitcast(self, dt):
        return View(self.ap.bitcast(dt), self.buf)

    def unsq(self, ax):
        return View(self.ap.unsqueeze(ax), self.buf)

    def on(self, buf):
        return View(self.ap, buf)


def _ap(v):
    return v.ap if isinstance(v, View) else v


class Prog:
    def __init__(self, nc, stack):
        self.nc = nc
        self.stack = stack
        self.q = {e: [] for e in ENGS}
        self.sem = {}
        self.cnt = {}
        self.waited = {e: {} for e in ENGS}
        self.nsem = 0
        for e in ENGS:
            self._new_sem(e)
        self.dma_events = {}
        self.sb_off = 16640
        self.sb_top = 229344
        self.ntile = 0

    def tile(self, shape, dt, name="t", nbuf=None):
        nbytes = int(np.prod(shape[1:])) * _DT_SIZE[dt]
        nbytes = (nbytes + 63) // 64 * 64
        off = self.sb_off
        self.sb_off += nbytes
        assert self.sb_off <= self.sb_top, f"SBUF overflow at {name}: {self.sb_off}"
        self.ntile += 1
        h = self.nc.alloc_sbuf_tensor_at(f"{name}_{self.ntile}", list(shape), dt, offset=off)
        return View(h.ap(), Buf(name))

    def psum(self, name):
        self.ntile += 1
        h = self.nc.alloc_psum_tensor(f"{name}_{self.ntile}", [128, 512], F32)
        return View(h.ap(), Buf(name))

    def _alloc_sem(self, name):
        self.nsem += 1
        return self.stack.enter_context(self.nc.semaphore(f"{name}_{self.nsem}"))

    def _new_sem(self, e):
        self.sem[e] = self._alloc_sem(f"s_{e}")
        self.cnt[e] = 0

    def wait(self, e, ev):
        sem, v = ev
        if self.waited[e].get(sem, 0) >= v:
            return
        self.waited[e][sem] = v
        self.q[e].append(lambda eng, sem=sem, v=v: eng.wait_ge(sem, v))

    def _deps(self, e, reads, writes):
        deps = {}

        def add(sem, v):
            if deps.get(sem, 0) < v:
                deps[sem] = v
        for b in reads:
            if b.w is not None:
                add(*b.w)
        for b in writes:
            if b.w is not None:
                add(*b.w)
            for sem, v in b.r.items():
                add(sem, v)
        for sem, v in deps.items():
            self.wait(e, (sem, v))

    def _record(self, ev, reads, writes):
        for b in reads:
            if b.r.get(ev[0], 0) < ev[1]:
                b.r[ev[0]] = ev[1]
        for b in writes:
            b.w = ev
            b.r = {}

    def emit(self, e, fn, reads=(), writes=()):
        reads = [b for b in reads if b is not None]
        writes = [b for b in writes if b is not None]
        self._deps(e, reads, writes)
        if self.cnt[e] >= 30000:
            self._new_sem(e)
        self.cnt[e] += 1
        ev = (self.sem[e], self.cnt[e])
        self.q[e].append(lambda eng, sem=ev[0]: fn(eng).then_inc(sem, 1))
        self._record(ev, reads, writes)
        return ev

    def emit_group(self, e, fns, reads, writes):
        self._deps(e, reads, writes)
        if self.cnt[e] >= 30000:
            self._new_sem(e)
        self.cnt[e] += 1
        ev = (self.sem[e], self.cnt[e])
        for fn in fns[:-1]:
            self.q[e].append(lambda eng, fn=fn: fn(eng))
        self.q[e].append(lambda eng, sem=ev[0], fn=fns[-1]: fn(eng).then_inc(sem, 1))
        self._record(ev, reads, writes)
        return ev

    def dma(self, e, out, in_, sb):
        b = sb.buf
        reads = [in_.buf] if isinstance(in_, View) else []
        writes = [out.buf] if isinstance(out, View) else []
        self._deps(e, reads, writes)
        if b.dsem is None:
            b.dsem = self._alloc_sem("d_" + b.name)
        b.dcnt += 16
        ev = (b.dsem, b.dcnt)
        o, i = _ap(out), _ap(in_)
        self.q[e].append(lambda eng, o=o, i=i, sem=ev[0]: eng.dma_start(out=o, in_=i).then_inc(sem, 16))
        self._record(ev, reads, writes)
        self.dma_events[ev[0]] = ev[1]
        return ev

    def idma(self, out, in_, idx, sb, scatter):
        e = "pool"
        b = sb.buf
        reads = [idx.buf] + ([in_.buf] if isinstance(in_, View) else [])
        writes = [out.buf] if isinstance(out, View) else []
        self._deps(e, reads, writes)
        if b.dsem is None:
            b.dsem = self._alloc_sem("d_" + b.name)
        b.dcnt += 16
        ev = (b.dsem, b.dcnt)
        o, i, ix = _ap(out), _ap(in_), idx.ap

        def th(eng, o=o, i=i, ix=ix, sem=ev[0], scatter=scatter):
            off = bass.IndirectOffsetOnAxis(ap=ix, axis=0)
            if scatter:
                ins = eng.indirect_dma_start(out=o, out_offset=off, in_=i, in_offset=None)
            else:
                ins = eng.indirect_dma_start(out=o, out_offset=None, in_=i, in_offset=off)
            ins.then_inc(sem, 16)
        self.q[e].append(th)
        self._record(ev, reads, writes)
        self.dma_events[ev[0]] = ev[1]
        return ev

    def barrier(self):
        evs = [(self.sem[e], self.cnt[e]) for e in ENGS if self.cnt[e] > 0]
        evs += list(self.dma_events.items())
        for e in ENGS:
            for ev in evs:
                self.wait(e, ev)
        self.dma_events = {}

    def act(self, out, in_, func, scale=1.0, bias=0.0, accum=None):
        reads = [in_.buf] + [v.buf for v in (scale, bias) if isinstance(v, View)]
        writes = [out.buf] + ([accum.buf] if accum is not None else [])
        kw = dict(out=out.ap, in_=in_.ap, func=func, scale=_ap(scale), bias=_ap(bias))
        if accum is not None:
            kw["accum_out"] = accum.ap
        return self.emit("act", lambda e: e.activation(**kw), reads, writes)

    def tt(self, out, a, b, op, eng="dve"):
        return self.emit(eng, lambda e: e.tensor_tensor(out=out.ap, in0=a.ap, in1=b.ap, op=op),
                         [a.buf, b.buf], [out.buf])

    def ts(self, out, a, s1, s2, op0, op1=None, eng="dve", accum=None):
        reads = [a.buf] + [v.buf for v in (s1, s2) if isinstance(v, View)]
        writes = [out.buf] + ([accum.buf] if accum is not None else [])
        kw = dict(out=out.ap, in0=a.ap, scalar1=_ap(s1), scalar2=_ap(s2), op0=op0)
        if op1 is not None:
            kw["op1"] = op1
        if accum is not None:
            kw["accum_out"] = accum.ap
        return self.emit(eng, lambda e: e.tensor_scalar(**kw), reads, writes)

    def stt(self, out, a, s, b, op0, op1, accum=None):
        reads = [a.buf, b.buf] + ([s.buf] if isinstance(s, View) else [])
        writes = [out.buf] + ([accum.buf] if accum is not None else [])
        kw = dict(out=out.ap, in0=a.ap, scalar=_ap(s), in1=b.ap, op0=op0, op1=op1)
        if accum is not None:
            kw["accum_out"] = accum.ap
        return self.emit("dve", lambda e: e.scalar_tensor_tensor(**kw), reads, writes)

    def copy(self, out, a, eng="dve"):
        if eng == "act":
            return self.emit("act", lambda e: e.copy(out=out.ap, in_=a.ap), [a.buf], [out.buf])
        return self.emit(eng, lambda e: e.tensor_copy(out=out.ap, in_=a.ap), [a.buf], [out.buf])

    def memset(self, out, val, eng="pool"):
        return self.emit(eng, lambda e: e.memset(out.ap, val), [], [out.buf])

    def reduce(self, out, a, op=ALU.add):
        return self.emit("dve", lambda e: e.tensor_reduce(out=out.ap, in_=a.ap, axis=AX.X, op=op),
                         [a.buf], [out.buf])

    def recip(self, out, a):
        return self.emit("dve", lambda e: e.reciprocal(out=out.ap, in_=a.ap), [a.buf], [out.buf])

    def rsqrt(self, out, a, nh):
        return self.emit("pool", lambda e: e.tensor_tensor(out=out.ap, in0=a.ap, in1=nh.ap, op=ALU.pow),
                         [a.buf, nh.buf], [out.buf])

    def pe(self, items):
        fns, reads, writes = [], [], []
        for it in items:
            if it[0] == "mm":
                _, out, l, r, st, sp = it
                fns.append(lambda e, out=out, l=l, r=r, st=st, sp=sp:
                           e.matmul(out.ap, l.ap, r.ap, start=st, stop=sp))
                reads += [l.buf, r.buf]
                writes.append(out.buf)
            else:
                _, out, a, idn = it
                fns.append(lambda e, out=out, a=a, idn=idn: e.transpose(out.ap, a.ap, idn.ap))
                reads += [a.buf, idn.buf]
                writes.append(out.buf)
        reads = list({id(b): b for b in reads}.values())
        writes = list({id(b): b for b in writes}.values())
        return self.emit_group("pe", fns, reads, writes)

    def finish(self):
        nc = self.nc
        q = self.q
        with nc.Block() as block:
            @block.sync
            def _(eng):
                for th in q["sp"]:
                    th(eng)

            @block.scalar
            def _(eng):
                for th in q["act"]:
                    th(eng)

            @block.vector
            def _(eng):
                for th in q["dve"]:
                    th(eng)

            @block.gpsimd
            def _(eng):
                for th in q["pool"]:
                    th(eng)

            @block.tensor
            def _(eng):
                for th in q["pe"]:
                    th(eng)


def build_program(stop_after=None, n_tiles_dbg=None):
    nc = bass.Bass("TRN2", target_bir_lowering=False)
    stack = contextlib.ExitStack()
    P = Prog(nc, stack)

    def din(name, shape, dt=F32):
        return nc.dram_tensor(name, list(shape), dt, kind="ExternalInput").ap()

    x_d = din("x", [S, D])
    cT_d = din("cT", [128, 8])
    pos_d = din("pos", [128, NT], I32)
    invf_d = din("invf", [128, 32])
    ident_d = din("ident", [128, 128])
    tri_d = din("tri", [128, 128])
    ada_w_d = din("ada_w", [2, D, 6 * D])
    ada_b_d = din("ada_b", [2, 6 * D])
    nmix_d = din("nmix_b", [2, 128, D])
    nffn_d = din("nffn_b", [2, 128, D])
    w_in_d = din("w_in", [2, D, D_IN])
    sgn_d = din("sgn_b", [2, 128, 512])
    sguT_d = din("sguT", [2, 128, 8, 128])
    sgb_d = din("sgb", [2, 128, 512])
    qln_d = din("qln_b", [2, 128, 256])
    kvln_d = din("kvln_b", [2, 128, 128])
    w_uq_d = din("w_uq", [2, 256, 768])
    w_ukv_d = din("w_ukv", [2, 128, 1024])
    qn_d = din("qn_b", [2, 128, 192])
    kn_d = din("kn_b", [2, 128, 192])
    w_out_d = din("w_out", [2, D, D])
    fw1L_d = din("fw1L", [11 * 128, 2048])
    fw3L_d = din("fw3L", [11 * 128, 2048])
    fw2L_d = din("fw2L", [128, NF * D])
    rw_d = din("router_w", [1, D, NE])
    SPARSE = True
    if SPARSE:
        w1L_d = din("w1L", [NE * 11 * 128, 2048])
        w3L_d = din("w3L", [NE * 11 * 128, 2048])
        w2L_d = din("w2L", [NE * 128 * 2, 11 * D])
        ebase_d = din("ebase", [128, NE])
        giota_d = din("giota", [128, 18])
        cth_d = din("ct_h", [128, 8])
        ctw_d = din("ct_w", [128, 11])
        ctw2_d = din("ct_w2", [128, 2])
        hs_d = nc.dram_tensor("hs", [NE * 4608, D], BF16, kind="Internal").ap()
        ys_d = nc.dram_tensor("ys", [NE * 4608, D], F32, kind="Internal").ap()
    else:
        moe_w1_d = din("moe_w1", [1, NE, D, DFF])
        moe_w3_d = din("moe_w3", [1, NE, D, DFF])
        moe_w2_d = din("moe_w2", [1, NE, DFF, D])
    y_d = nc.dram_tensor("y", [S, D], F32, kind="ExternalOutput").ap()
    xs_d = nc.dram_tensor("xs", [S, D], F32, kind="Internal").ap()

    nt_run = NT if n_tiles_dbg is None else n_tiles_dbg

    PS = [P.psum(f"ps{i}") for i in range(8)]

    ident = P.tile([128, 128], BF16, "ident")
    tri = P.tile([128, 128], BF16, "tri")
    ones = P.tile([128, 128], BF16, "ones")
    ident32 = P.tile([128, 128], F32, "ident32")
    nhalf = P.tile([128, 8], F32, "nhalf")
    cosT = P.tile([128, NT, 32], BF16, "cos")
    sinT = P.tile([128, NT, 32], BF16, "sin")
    cact = P.tile([128, 8], F32, "cact")
    ones1 = P.tile([1, 128], F32, "ones1")
    modb = P.tile([128, 3, D], BF16, "modb")
    persist_mark = P.sb_off

    P.dma("pool", ident, ident_d, ident)
    P.dma("pool", tri, tri_d, tri)
    P.dma("sp", ident32, ident_d, ident32)
    P.memset(ones, 1.0)
    P.memset(nhalf, -0.5)
    P.memset(ones1, 1.0)
    m0 = P.sb_off
    cT = P.tile([128, 8], F32, "cT")
    posi = P.tile([128, NT], I32, "posi")
    posf = P.tile([128, NT], F32, "posf")
    invf = P.tile([128, 32], F32, "invf")
    ang = P.tile([128, NT, 32], F32, "ang")
    ang2 = P.tile([128, NT, 32], F32, "ang2")
    P.dma("sp", cT, cT_d, cT)
    P.dma("sp", posi, pos_d, posi)
    P.dma("sp", invf, invf_d, invf)
    P.act(cact, cT, AF.Silu)
    P.copy(posf, posi)
    P.tt(ang, posf.unsq(2).bc([128, NT, 32]), invf.unsq(1).bc([128, NT, 32]), ALU.mult)
    angi = P.tile([128, NT, 32], I32, "angi")
    angf = P.tile([128, NT, 32], F32, "angf")
    msk = P.tile([128, NT, 32], F32, "msk")

    def sin_of(dst, shift):
        P.ts(ang2, ang, float(1.0 / (2.0 * np.pi)), float(shift), ALU.mult, ALU.add)
        P.copy(angi, ang2)
        P.copy(angf, angi)
        P.tt(ang2, ang2, angf, ALU.subtract)
        P.ts(msk, ang2, 0.5, None, ALU.is_gt)
        P.tt(ang2, ang2, msk, ALU.subtract)
        P.ts(msk, ang2, -0.5, None, ALU.is_lt)
        P.tt(ang2, ang2, msk, ALU.add)
        P.act(dst, ang2, AF.Sin, scale=6.283185)

    sin_of(sinT, 0.0)
    sin_of(cosT, 0.25)
    P.barrier()
    P.sb_off = m0

    def compute_mod(layer, sub, norm_d):
        m = P.sb_off
        wbuf = [P.tile([128, 3 * D], F32, f"adaw{i}") for i in range(2)]
        crep = P.tile([128, 8, 128], F32, "crep")
        for k in range(8):
            P.copy(crep[:, k, :], cact[:, k:k + 1].bc([128, 128]))
        brow = P.tile([1, 3 * D], F32, "brow")
        gb = P.tile([128, D], F32, "gb")
        t1 = P.tile([128, D], F32, "t1")
        c0 = sub * 3 * D
        P.dma("sp", brow, ada_b_d[layer:layer + 1, c0:c0 + 3 * D], brow)
        P.dma("sp", gb, norm_d[layer], gb)
        for k in range(8):
            wb = wbuf[k % 2]
            P.dma("sp", wb, ada_w_d[layer, k * 128:(k + 1) * 128, c0:c0 + 3 * D], wb)
            for j in range(6):
                P.pe([("mm", PS[j], crep[:, k, :], wb[:, j * 512:(j + 1) * 512], k == 0, False)])
        for j in range(6):
            P.pe([("mm", PS[j], ones1, brow[:, j * 512:(j + 1) * 512], False, True)])
        for hh in range(2):
            P.stt(t1[:, hh * 512:(hh + 1) * 512], PS[2 + hh], 1.0, gb[:, hh * 512:(hh + 1) * 512],
                  ALU.add, ALU.mult)
            P.copy(modb[:, 1, hh * 512:(hh + 1) * 512], PS[0 + hh], eng="act")
            P.copy(modb[:, 2, hh * 512:(hh + 1) * 512], PS[4 + hh], eng="act")
        P.copy(modb[:, 0, :], t1)
        P.barrier()
        P.sb_off = m

    def norm_tile(xt, hT_dst, tmp, h32=None):
        junk, ss, ms, rstd, hbf, tpsum = tmp
        P.act(junk, xt, AF.Square, accum=ss)
        P.ts(ms, ss, 1.0 / D, EPS, ALU.mult, ALU.add)
        P.rsqrt(rstd, ms, nhalf[:, 0:1])
        hdst = h32 if h32 is not None else junk
        P.stt(hdst, xt, rstd, modb[:, 0, :], ALU.mult, ALU.mult)
        if h32 is not None:
            P.tt(h32, h32, modb[:, 1, :], ALU.add)
            P.copy(hbf, h32, eng="act")
        else:
            P.tt(hbf, junk, modb[:, 1, :], ALU.add)
        tp = tpsum.bitcast(BF16)
        P.pe([("tr", tp[:, j * 128:(j + 1) * 128], hbf[:, j * 128:(j + 1) * 128], ident) for j in range(8)])
        P.copy(hT_dst, tp.re("p (k t) -> p k t", k=8), eng="act")

    def mixer(layer, src_d, dst_d):
        m = P.sb_off
        Win = P.tile([128, 8, D_IN], BF16, "Win")
        Wuq = P.tile([128, 2, 768], BF16, "Wuq")
        Wukv = P.tile([128, 1024], BF16, "Wukv")
        Wout = P.tile([128, 8, D], BF16, "Wout")
        WcT = P.tile([128, 8, 128], BF16, "WcT")
        sgn = P.tile([128, 512], BF16, "sgn")
        sgb = P.tile([128, 512], BF16, "sgb")
        qln = P.tile([128, 256], F32, "qln")
        kvln = P.tile([128, 128], F32, "kvln")
        qn = P.tile([128, 192], F32, "qn")
        kn = P.tile([128, 192], F32, "kn")
        GQ = P.tile([128, 128], F32, "GQ")
        P.dma("pool", Win, w_in_d[layer].rearrange("(k p) n -> p k n", p=128), Win)
        P.dma("pool", Wuq, w_uq_d[layer].rearrange("(k p) n -> p k n", p=128), Wuq)
        P.dma("pool", Wukv, w_ukv_d[layer], Wukv)
        P.dma("pool", Wout, w_out_d[layer].rearrange("(k p) n -> p k n", p=128), Wout)
        P.dma("pool", WcT, sguT_d[layer], WcT)
        P.dma("pool", sgn, sgn_d[layer], sgn)
        P.dma("pool", sgb, sgb_d[layer], sgb)
        P.dma("sp", qln, qln_d[layer], qln)
        P.dma("sp", kvln, kvln_d[layer], kvln)
        P.dma("sp", qn, qn_d[layer], qn)
        P.dma("sp", kn, kn_d[layer], kn)
        compute_mod(layer, 0, nmix_d)
        P.tt(WcT, WcT, tri.unsq(1).bc([128, 8, 128]), ALU.mult)
        P.tt(Wout, Wout, modb[:, 2, :].unsq(1).bc([128, 8, D]), ALU.mult)
        P.tt(GQ, qn[:, 0:128], kn[:, 0:128], ALU.mult)
        KTt = P.tile([128, 4, S], BF16, "KT")
        RTt = P.tile([64, S], BF16, "RT")
        Vt = P.tile([128, NT, 512], BF16, "V")
        rstdk = P.tile([128, NT, 4], F32, "rstdk")
        kbufs = [Buf(f"kblk{j}") for j in range(8)]
        hT = P.tile([128, 8, 512], BF16, "hT")
        QTn = P.tile([128, 4, 512], BF16, "QTn")
        QTr = P.tile([64, 4, 512], BF16, "QTr")
        catT = P.tile([128, 8, 512], BF16, "catT")
        xts = [P.tile([128, D], F32, f"xt{i}") for i in range(2)]
        junk = P.tile([128, D], F32, "junk")
        hbf = P.tile([128, D], BF16, "hbf")
        ss = P.tile([128, 1], F32, "ss")
        ms = P.tile([128, 1], F32, "ms")
        rstd = P.tile([128, 1], F32, "rstd")
        gu = P.tile([128, 512], BF16, "gu")
        gv = P.tile([128, 512], F32, "gv")
        st6 = P.tile([128, 6], F32, "st6")
        mv = P.tile([128, 2], F32, "mv")
        rv = P.tile([128, 1], F32, "rv")
        vtmp = P.tile([128, 512], F32, "vtmp")
        vb = P.tile([128, 512], BF16, "vb")
        stmp = vtmp
        ab = P.tile([128, 512], BF16, "ab")
        lat = P.tile([128, 448], F32, "lat")
        sl = P.tile([128, 4], F32, "sl")
        latn = P.tile([128, 384], BF16, "latn")
        latT = P.tile([128, 3, 128], BF16, "latT")
        qf = P.tile([128, 768], F32, "qf")
        sq = P.tile([128, 768], F32, "sq")
        ssq = P.tile([128, 4], F32, "ssq")
        rq = P.tile([128, 4], F32, "rq")
        qnb = P.tile([128, 4, 128], BF16, "qnb")
        qrb = P.tile([128, 4, 64], BF16, "qrb")
        r1 = P.tile([128, 4, 64], F32, "r1")
        r2 = P.tile([128, 4, 64], F32, "r2")
        r3 = P.tile([128, 4, 64], F32, "r3")
        knb = P.tile([128, 4, 128], BF16, "knb")
        ssk = P.tile([128, 4], F32, "ssk")
        sskr = P.tile([128, 1], F32, "sskr")
        krb = P.tile([128, 64], BF16, "krb")
        k1 = P.tile([128, 64], F32, "k1")
        k2 = P.tile([128, 64], F32, "k2")
        k3 = P.tile([128, 64], F32, "k3")
        Es = [P.tile([128, 512], BF16, f"E{i}") for i in range(2)]
        rec = gv
        xr = [P.tile([128, D], F32, f"xr{i}") for i in range(2)]
        ytmp = junk

        nblk = (nt_run + 3) // 4
        for j in range(nblk):
            kb = kbufs[j]
            def g_prelude(t, ti):
                X = (PS[1], PS[2], PS[3]) if ti % 2 == 0 else (PS[4], PS[5], PS[6])
                c0 = ti * 128
                xt = xts[t % 2]
                P.dma("sp", xt, src_d[t * 128:(t + 1) * 128, :], xt); yield
                P.act(junk, xt, AF.Square, accum=ss); yield
                P.ts(ms, ss, 1.0 / D, EPS, ALU.mult, ALU.add); yield
                P.rsqrt(rstd, ms, nhalf[:, 0:1]); yield
                P.stt(junk, xt, rstd, modb[:, 0, :], ALU.mult, ALU.mult); yield
                P.tt(hbf, junk, modb[:, 1, :], ALU.add); yield
                tp = X[0].bitcast(BF16)
                P.pe([("tr", tp[:, jj * 128:(jj + 1) * 128], hbf[:, jj * 128:(jj + 1) * 128], ident) for jj in range(8)]); yield
                P.copy(hT[:, :, c0:c0 + 128], tp.re("p (k t) -> p k t", k=8), eng="act"); yield
                P.pe([("mm", X[2][:, 0:448], hT[:, k, c0:c0 + 128], Win[:, k, 1024:1472], k == 0, k == 7) for k in range(8)]); yield
                P.pe([("mm", X[1], hT[:, k, c0:c0 + 128], Win[:, k, 512:1024], k == 0, k == 7) for k in range(8)]); yield
                P.pe([("mm", X[0], hT[:, k, c0:c0 + 128], Win[:, k, 0:512], k == 0, k == 7) for k in range(8)]); yield

            for ti in range(4):
                t = 4 * j + ti
                c0 = ti * 128
                X = (PS[1], PS[2], PS[3]) if ti % 2 == 0 else (PS[4], PS[5], PS[6])
                if ti == 0:
                    for _ in g_prelude(t, ti):
                        pass
                sqk = Es[0].bitcast(F32)
                state = {"lat": False}

                def g_sgu(c0=c0):
                    P.act(gu, X[0], AF.Gelu_apprx_tanh); yield
                    P.act(gv, X[1], AF.Gelu_apprx_tanh); yield
                    P.emit("dve", lambda e: e.bn_stats(out=st6.ap, in_=gv.ap), [gv.buf], [st6.buf]); yield
                    P.emit("dve", lambda e: e.bn_aggr(out=mv.ap, in_=st6.ap), [st6.buf], [mv.buf]); yield
                    P.ts(rv, mv[:, 1:2], EPS, None, ALU.add); yield
                    P.rsqrt(rv, rv, nhalf[:, 0:1]); yield
                    P.ts(vtmp, gv, mv[:, 0:1], rv, ALU.subtract, ALU.mult); yield
                    P.tt(vb, vtmp, sgn, ALU.mult); yield
                    P.pe([("mm", PS[0][:, g * 64:(g + 1) * 64], WcT[:, g, :], vb[:, g * 64:(g + 1) * 64], True, True)
                          for g in range(8)]); yield
                    P.tt(stmp, PS[0], sgb, ALU.add); yield
                    P.tt(ab, stmp, gu, ALU.mult); yield
                    tp = PS[0].bitcast(BF16)
                    P.pe([("tr", tp[:, g * 128:(g + 1) * 128], ab[:, g * 128:(g + 1) * 128], ident) for g in range(4)]); yield
                    P.copy(catT[:, 0:4, c0:c0 + 128], tp[:, 0:512].re("p (k t) -> p k t", k=4), eng="act"); yield

                def g_latq(c0=c0, t=t):
                    P.copy(lat, X[2][:, 0:448], eng="act"); yield
                    P.stt(sq[:, 0:256], lat[:, 0:256], 1.0, lat[:, 0:256], ALU.mult, ALU.mult, accum=sl[:, 0:1]); yield
                    P.stt(sq[:, 256:384], lat[:, 256:384], 1.0, lat[:, 256:384], ALU.mult, ALU.mult, accum=sl[:, 1:2]); yield
                    P.stt(sq[:, 384:448], lat[:, 384:448], 1.0, lat[:, 384:448], ALU.mult, ALU.mult, accum=sskr); yield
                    P.ts(sl[:, 2:3], sl[:, 0:1], 1.0 / 256, EPS, ALU.mult, ALU.add); yield
                    P.ts(sl[:, 3:4], sl[:, 1:2], 1.0 / 128, EPS, ALU.mult, ALU.add); yield
                    P.rsqrt(sl[:, 2:4], sl[:, 2:4], nhalf[:, 0:2]); yield
                    P.stt(latn[:, 0:256], lat[:, 0:256], sl[:, 2:3], qln, ALU.mult, ALU.mult); yield
                    P.stt(latn[:, 256:384], lat[:, 256:384], sl[:, 3:4], kvln, ALU.mult, ALU.mult); yield
                    tp = X[2].bitcast(BF16)
                    P.pe([("tr", tp[:, g * 128:(g + 1) * 128], latn[:, g * 128:(g + 1) * 128], ident) for g in range(3)]); yield
                    P.copy(latT, tp[:, 0:384].re("p (k t) -> p k t", k=3), eng="act"); yield
                    P.pe([("mm", X[2], latT[:, 2, :], Wukv[:, 0:512], True, True),
                          ("mm", PS[7], latT[:, 2, :], Wukv[:, 512:1024], True, True)])
                    state["lat"] = True
                    yield
                    P.pe([("mm", X[0], latT[:, 0, :], Wuq[:, 0, 0:512], True, False),
                          ("mm", X[0], latT[:, 1, :], Wuq[:, 1, 0:512], False, True),
                          ("mm", X[1][:, 0:256], latT[:, 0, :], Wuq[:, 0, 512:768], True, False),
                          ("mm", X[1][:, 0:256], latT[:, 1, :], Wuq[:, 1, 512:768], False, True)]); yield
                    P.copy(qf[:, 0:512], X[0], eng="act"); yield
                    P.copy(qf[:, 512:768], X[1][:, 0:256], eng="act"); yield
                    P.tt(sq, qf, qf, ALU.mult); yield
                    P.reduce(ssq, sq.re("p (h d) -> p h d", h=4)); yield
                    P.ts(ssq, ssq, 1.0 / 192, EPS, ALU.mult, ALU.add); yield
                    P.rsqrt(rq, ssq, nhalf[:, 0:4]); yield
                    q3 = qf.re("p (h d) -> p h d", h=4)
                    sq3 = sq.re("p (h d) -> p h d", h=4)
                    P.tt(sq3[:, :, 0:128], q3[:, :, 0:128], GQ.unsq(1).bc([128, 4, 128]), ALU.mult); yield
                    P.tt(qnb, sq3[:, :, 0:128], rq.unsq(2).bc([128, 4, 128]), ALU.mult); yield
                    tp = X[0].bitcast(BF16)
                    P.pe([("tr", tp[:, h * 128:(h + 1) * 128], qnb[:, h, :], ident) for h in range(4)]); yield
                    P.copy(QTn[:, :, c0:c0 + 128], tp[:, 0:512].re("p (k t) -> p k t", k=4), eng="act"); yield
                    cs = cosT[:, t, :].unsq(1).bc([128, 4, 32])
                    sn = sinT[:, t, :].unsq(1).bc([128, 4, 32])
                    P.tt(r1, q3[:, :, 128:192], qn[:, 128:192].unsq(1).bc([128, 4, 64]), ALU.mult); yield
                    P.tt(r2[:, :, 0:32], r1[:, :, 0:32], cs, ALU.mult); yield
                    P.tt(r2[:, :, 32:64], r1[:, :, 32:64], cs, ALU.mult); yield
                    P.tt(r3[:, :, 0:32], r1[:, :, 32:64], sn, ALU.mult); yield
                    P.tt(r3[:, :, 32:64], r1[:, :, 0:32], sn, ALU.mult); yield
                    P.tt(r2[:, :, 0:32], r2[:, :, 0:32], r3[:, :, 0:32], ALU.subtract); yield
                    P.tt(r2[:, :, 32:64], r2[:, :, 32:64], r3[:, :, 32:64], ALU.add); yield
                    P.tt(qrb, r2, rq.unsq(2).bc([128, 4, 64]), ALU.mult); yield
                    tp = X[1].bitcast(BF16)
                    P.pe([("tr", tp[0:64, h * 128:(h + 1) * 128], qrb[:, h, :], ident) for h in range(4)]); yield
                    P.copy(QTr[:, :, c0:c0 + 128], tp[0:64, 0:512].re("p (k t) -> p k t", k=4), eng="act"); yield

                def g_kv(t=t, kb=kb):
                    while not state["lat"]:
                        yield
                    for half in range(2):
                        kv3 = (X[2] if half == 0 else PS[7]).re("p (h c) -> p h c", h=2)
                        P.copy(Vt[:, t, half * 256:(half + 1) * 256].re("p (h d) -> p h d", h=2).on(kb),
                               kv3[:, :, 128:256], eng="act"); yield
                        P.copy(knb[:, 2 * half:2 * half + 2, :], kv3[:, :, 0:128], eng="act"); yield
                        P.tt(sqk.re("p (h d) -> p h d", h=2), kv3[:, :, 0:128], knb[:, 2 * half:2 * half + 2, :],
                             ALU.mult); yield
                        P.reduce(ssk[:, 2 * half:2 * half + 2], sqk.re("p (h d) -> p h d", h=2)); yield
                    P.ts(ssk, ssk, sskr, 1.0 / 192, ALU.add, ALU.mult); yield
                    P.ts(ssk, ssk, EPS, None, ALU.add); yield
                    P.rsqrt(ssk, ssk, nhalf[:, 0:4]); yield
                    P.ts(rstdk[:, t, :].on(kb), ssk, float(192 ** -0.5), None, ALU.mult); yield
                    tp = PS[7].bitcast(BF16)
                    P.pe([("tr", tp[:, h * 128:(h + 1) * 128], knb[:, h, :], ident) for h in range(4)]); yield
                    P.copy(KTt[:, :, t * 128:(t + 1) * 128].on(kb), tp[:, 0:512].re("p (k t) -> p k t", k=4), eng="act"); yield
                    cs1 = cosT[:, t, :]
                    sn1 = sinT[:, t, :]
                    P.tt(k1, lat[:, 384:448], kn[:, 128:192], ALU.mult, eng="pool"); yield
                    P.tt(k2[:, 0:32], k1[:, 0:32], cs1, ALU.mult, eng="pool"); yield
                    P.tt(k2[:, 32:64], k1[:, 32:64], cs1, ALU.mult, eng="pool"); yield
                    P.tt(k3[:, 0:32], k1[:, 32:64], sn1, ALU.mult, eng="pool"); yield
                    P.tt(k3[:, 32:64], k1[:, 0:32], sn1, ALU.mult, eng="pool"); yield
                    P.tt(krb[:, 0:32], k2[:, 0:32], k3[:, 0:32], ALU.subtract, eng="pool"); yield
                    P.tt(krb[:, 32:64], k2[:, 32:64], k3[:, 32:64], ALU.add, eng="pool"); yield
                    tp = PS[7].bitcast(BF16)
                    P.pe([("tr", tp[0:64, 0:128], krb, ident)]); yield
                    P.copy(RTt[:, t * 128:(t + 1) * 128].on(kb), tp[0:64, 0:128], eng="act"); yield

                gens = [g_sgu(), g_latq(), g_kv()]
                if ti < 3:
                    gens.insert(0, g_prelude(t + 1, ti + 1))
                while gens:
                    for g in list(gens):
                        try:
                            next(g)
                        except StopIteration:
                            gens.remove(g)

            nkt = 4 * (j + 1)
            units = [(h, kt) for h in range(4) for kt in range(nkt)]
            Sb = [PS[0], PS[1]]

            def emit_S(i):
                h, kt = units[i]
                kbk = kbufs[kt // 4]
                qoff = max(0, kt - 4 * j) * 128
                sb = Sb[i % 2]
                P.pe([("mm", sb[:, qoff:512], KTt[:, h, kt * 128:(kt + 1) * 128].on(kbk), QTn[:, h, qoff:512], True, False),
                      ("mm", sb[:, qoff:512], RTt[:, kt * 128:(kt + 1) * 128].on(kbk), QTr[:, h, qoff:512], False, True)])

            emit_S(0)
            for i, (h, kt) in enumerate(units):
                if i + 1 < len(units):
                    emit_S(i + 1)
                kbk = kbufs[kt // 4]
                qoff = max(0, kt - 4 * j) * 128
                sb = Sb[i % 2]
                Ob = PS[2 + (h % 2)]
                Lb = PS[4 + (h % 2)]
                E = Es[i % 2]
                P.act(E[:, qoff:512], sb[:, qoff:512], AF.Exp, scale=rstdk[:, kt, h:h + 1].on(kbk))
                if kt >= 4 * j:
                    P.tt(E[:, qoff:qoff + 128], E[:, qoff:qoff + 128], tri, ALU.mult, eng="pool")
                P.pe([("mm", Ob[:, qoff:512], Vt[:, kt, h * 128:(h + 1) * 128].on(kbk), E[:, qoff:512], kt == 0, kt == nkt - 1),
                      ("mm", Lb[:, qoff:512], ones, E[:, qoff:512], kt == 0, kt == nkt - 1)])
                if kt == nkt - 1:
                    P.recip(rec, Lb)
                    P.tt(catT[:, 4 + h, :], Ob, rec, ALU.mult)

            P.dma("sp", xr[(4 * j) % 2], src_d[4 * j * 128:(4 * j + 1) * 128, :], xr[(4 * j) % 2])
            for ti in range(4):
                t = 4 * j + ti
                c0 = ti * 128
                x2 = xr[t % 2]
                if ti < 3:
                    P.dma("sp", xr[(t + 1) % 2], src_d[(t + 1) * 128:(t + 2) * 128, :], xr[(t + 1) % 2])
                items = []
                pa, pb = [(PS[6], PS[7]), (PS[0], PS[1]), (PS[2], PS[3]), (PS[4], PS[5])][ti]
                for k in range(8):
                    l = catT[:, k, c0:c0 + 128]
                    items.append(("mm", pa, l, Wout[:, k, 0:512], k == 0, k == 7))
                    items.append(("mm", pb, l, Wout[:, k, 512:1024], k == 0, k == 7))
                P.pe(items)
                o = x2
                P.tt(o[:, 0:512], pa, x2[:, 0:512], ALU.add)
                P.tt(o[:, 512:1024], pb, x2[:, 512:1024], ALU.add)
                P.dma("sp", dst_d[t * 128:(t + 1) * 128, :], o, o)
        P.barrier()
        P.sb_off = m

    def ffn(layer, src_d, dst_d, w1s, w3s, w2s, moe):
        m = P.sb_off
        if moe:
            compute_mod(layer, 1, nffn_d)
        nexp = len(w1s)
        TB = 1024
        hTs = [P.tile([128, 8, TB], BF16, "fhT0")]
        off_ov = P.sb_off
        if not moe:
            hTs.append(P.tile([128, 8, TB], BF16, "fhT1"))
        actT = P.tile([128, NF, TB], BF16, "actT")
        W2 = P.tile([128, NF, D], BF16, "W2")
        NWB = 4
        W1g = [P.tile([128, 8, 256], BF16, f"W1g{i}") for i in range(NWB)]
        W3g = [P.tile([128, 8, 256], BF16, f"W3g{i}") for i in range(NWB)]
        xts = [P.tile([128, D], F32, f"fx{i}") for i in range(2)]
        junk = P.tile([128, D], F32, "fjunk")
        hbf = P.tile([128, D], BF16, "fhbf")
        ss = P.tile([128, 1], F32, "fss")
        ms = P.tile([128, 1], F32, "fms")
        rstd = P.tile([128, 1], F32, "frstd")
        sg = [P.tile([128, 512], BF16, f"sg{i}") for i in range(2)]
        xr = [P.tile([128, D], F32, f"fxr{i}") for i in range(2)]
        ytmp = junk
        if moe:
            acc = P.tile([128, 8, D], F32, "acc")
            h32 = P.tile([128, D], F32, "h32")
            hT32 = P.tile([128, 8, 128], F32, "hT32")
            rw = P.tile([128, 8, NE], F32, "rw")
            lg = P.tile([128, NE], F32, "lg")
            lg2 = P.tile([128, NE], F32, "lg2")
            mk1 = P.tile([128, NE], F32, "mk1")
            mk2 = P.tile([128, NE], F32, "mk2")
            m1 = P.tile([128, 1], F32, "m1")
            m2 = P.tile([128, 1], F32, "m2")
            dd = P.tile([128, 1], F32, "dd")
            w1g = P.tile([128, 1], F32, "w1g")
            w2g = P.tile([128, 1], F32, "w2g")
            gates = P.tile([128, 8, NE], F32, "gates")
            P.dma("sp", rw, rw_d[0].rearrange("(k p) e -> p k e", p=128), rw)
        nblk = max(1, nt_run // 8)
        gi = 0
        hbf2 = [hbf, P.tile([128, D], BF16, "fhbf2")]

        def norm_a(blk, ti):
            t = blk * 8 + ti
            xt = xts[t % 2]
            hb = hbf2[ti % 2]
            P.dma("sp", xt, src_d[t * 128:(t + 1) * 128, :], xt)
            P.act(junk, xt, AF.Square, accum=ss)
            P.ts(ms, ss, 1.0 / D, EPS, ALU.mult, ALU.add)
            P.rsqrt(rstd, ms, nhalf[:, 0:1])
            P.stt(junk, xt, rstd, modb[:, 0, :], ALU.mult, ALU.mult)
            P.tt(hb, junk, modb[:, 1, :], ALU.add)

        def norm_b(blk, ti, bank):
            hb = hbf2[ti % 2]
            tp = PS[bank].bitcast(BF16)
            P.pe([("tr", tp[:, j * 128:(j + 1) * 128], hb[:, j * 128:(j + 1) * 128], ident) for j in range(8)])
            P.copy(hTs[blk % len(hTs)][:, :, ti * 128:(ti + 1) * 128], tp.re("p (k t) -> p k t", k=8), eng="act")

        def load_chunk(ci):
            if ci >= nblk * (NF // 2):
                return
            fgc = ci % (NF // 2)
            wa_, wb_ = W1g[ci % NWB], W3g[ci % NWB]
            P.dma("pool", wa_.re("p k c -> p (k c)"), w1s[0][fgc * 128:(fgc + 1) * 128, :], wa_)
            P.dma("pool", wb_.re("p k c -> p (k c)"), w3s[0][fgc * 128:(fgc + 1) * 128, :], wb_)

        if not moe:
            P.dma("pool", W2.re("p f n -> p (f n)"), w2s[0], W2)
            for pj in range(NWB - 1):
                load_chunk(pj)
            save_off = P.sb_off
            P.sb_off = off_ov
            compute_mod(layer, 1, nffn_d)
            P.sb_off = save_off
            for ti in range(8):
                norm_a(0, ti)
                if ti >= 1:
                    norm_b(0, ti - 1, 0)
            norm_b(0, 7, 0)
        for blk in range(nblk):
            hT = hTs[blk % len(hTs)]
            for ti in range(8 if moe else 0):
                t = blk * 8 + ti
                xt = xts[t % 2]
                P.dma("sp", xt, src_d[t * 128:(t + 1) * 128, :], xt)
                norm_tile(xt, hT[:, :, ti * 128:(ti + 1) * 128], (junk, ss, ms, rstd, hbf, PS[0]),
                          h32=h32 if moe else None)
                if moe:
                    for hh in range(2):
                        P.pe([("tr", PS[1 + hh][:, g * 128:(g + 1) * 128],
                               h32[:, (hh * 4 + g) * 128:(hh * 4 + g + 1) * 128], ident32) for g in range(4)])
                        P.copy(hT32[:, hh * 4:hh * 4 + 4, :], PS[1 + hh].re("p (k t) -> p k t", k=4), eng="act")
                    P.pe([("mm", PS[3][:, 0:NE], hT32[:, k, :], rw[:, k, :], k == 0, k == 7) for k in range(8)])
                    P.copy(lg, PS[3][:, 0:NE])
                    P.reduce(m1, lg, op=ALU.max)
                    P.ts(mk1, lg, m1, None, ALU.is_ge)
                    P.stt(lg2, mk1, -1e30, lg, ALU.mult, ALU.add)
                    P.reduce(m2, lg2, op=ALU.max)
                    P.ts(mk2, lg2, m2, None, ALU.is_ge)
                    P.tt(dd, m2, m1, ALU.subtract)
                    P.act(dd, dd, AF.Exp)
                    P.ts(dd, dd, 1.0, None, ALU.add)
                    P.recip(w1g, dd)
                    P.ts(w2g, w1g, -1.0, 1.0, ALU.mult, ALU.add)
                    P.ts(mk1, mk1, w1g, None, ALU.mult)
                    P.stt(gates[:, ti, :], mk2, w2g, mk1, ALU.mult, ALU.add)
            for e in range(nexp):
                w1d, w3d, w2d = w1s[e], w3s[e], w2s[e]
                if moe:
                    P.dma("pool", W2, w2d.rearrange("(f p) n -> p f n", p=128), W2)
                elif blk > 0:
                    P.dma("pool", W2.re("p f n -> p (f n)"), w2d, W2)
                if not moe:
                    P.tt(W2, W2, modb[:, 2, :].unsq(1).bc([128, NF, D]), ALU.mult)
                for fg in range(NF // 2):
                    wa, wb = W1g[gi % NWB], W3g[gi % NWB]
                    if moe:
                        P.dma("pool", wa, w1d[:, fg * 256:(fg + 1) * 256].rearrange("(k p) n -> p k n", p=128), wa)
                        P.dma("pool", wb, w3d[:, fg * 256:(fg + 1) * 256].rearrange("(k p) n -> p k n", p=128), wb)
                    else:
                        load_chunk(gi + NWB - 1)
                    gi += 1
                    for fi in range(2):
                        f = fg * 2 + fi
                        for sub in range(2):
                            gp = PS[(2 * sub) % 4]
                            up = PS[(2 * sub + 1) % 4]
                            tok = slice(sub * 512, (sub + 1) * 512)
                            P.pe([("mm", gp, wa[:, k, fi * 128:(fi + 1) * 128], hT[:, k, tok], k == 0, k == 7)
                                  for k in range(8)])
                            P.pe([("mm", up, wb[:, k, fi * 128:(fi + 1) * 128], hT[:, k, tok], k == 0, k == 7)
                                  for k in range(8)])
                            s_ = sg[sub]
                            P.act(s_, gp, AF.Silu)
                            P.tt(actT[:, f, tok], s_, up, ALU.mult)
                    if (not moe) and blk + 1 < nblk:
                        if 1 <= fg <= 8:
                            norm_b(blk + 1, fg - 1, 7)
                        if fg < 8:
                            norm_a(blk + 1, fg)
                for ti in range(8):
                    t = blk * 8 + ti
                    c0 = ti * 128
                    pa, pb = PS[4 + 2 * (ti % 2)], PS[5 + 2 * (ti % 2)]
                    items = []
                    for f in range(NF):
                        l = actT[:, f, c0:c0 + 128]
                        items.append(("mm", pa, l, W2[:, f, 0:512], f == 0, f == NF - 1))
                        items.append(("mm", pb, l, W2[:, f, 512:1024], f == 0, f == NF - 1))
                    P.pe(items)
                    if moe:
                        gcol = gates[:, ti, e:e + 1]
                        if e == 0:
                            P.ts(acc[:, ti, 0:512], pa, gcol, None, ALU.mult)
                            P.ts(acc[:, ti, 512:1024], pb, gcol, None, ALU.mult)
                        else:
                            P.stt(acc[:, ti, 0:512], pa, gcol, acc[:, ti, 0:512], ALU.mult, ALU.add)
                            P.stt(acc[:, ti, 512:1024], pb, gcol, acc[:, ti, 512:1024], ALU.mult, ALU.add)
                    if (not moe) or e == nexp - 1:
                        x2 = xr[t % 2]
                        if ti == 0:
                            P.dma("sp", x2, src_d[t * 128:(t + 1) * 128, :], x2)
                        if ti < 7:
                            P.dma("sp", xr[(t + 1) % 2], src_d[(t + 1) * 128:(t + 2) * 128, :], xr[(t + 1) % 2])
                        o = x2
                        if moe:
                            P.tt(ytmp, acc[:, ti, :], modb[:, 2, :], ALU.mult, eng="pool")
                            P.tt(o, ytmp, x2, ALU.add, eng="pool")
                        else:
                            P.tt(o[:, 0:512], pa, x2[:, 0:512], ALU.add)
                            P.tt(o[:, 512:1024], pb, x2[:, 512:1024], ALU.add)
                        P.dma("sp", dst_d[t * 128:(t + 1) * 128, :], o, o)
        P.barrier()
        P.sb_off = m

    def moe_sparse(layer, src_d, dst_d):
        m = P.sb_off
        compute_mod(layer, 1, nffn_d)
        TB = 768
        TPG = TB // 128
        NG = 18
        ESTR = 6 * TB
        xts = [P.tile([128, D], F32, f"sx{i}") for i in range(2)]
        hbfs = [P.tile([128, D], BF16, f"shbf{i}") for i in range(2)]
        ysb = [P.tile([128, D], F32, f"sys{i}") for i in range(4)]
        gw = P.tile([128, NT, 2], F32, "sgw")
        ridx = P.tile([128, NT, 2], I32, "sridx")
        iH = P.tile([128, NG, TPG], I32, "siH")
        iW = P.tile([128, NG, 11], I32, "siW")
        iW2 = P.tile([128, NG, 2], I32, "siW2")
        mark = P.sb_off
        rw = P.tile([128, 8, NE], F32, "srw")
        runc = P.tile([128, NE], F32, "srunc")
        ebase = P.tile([128, NE], F32, "sebase")
        ustr = P.tile([128, 128], BF16, "sustr")
        giota = P.tile([128, NG], F32, "sgiota")
        cth = P.tile([128, 8], F32, "scth")
        ctw = P.tile([128, 11], F32, "sctw")
        ctw2 = P.tile([128, 2], F32, "sctw2")
        ng = P.tile([128, NE], F32, "sng")
        cum = P.tile([128, NE], F32, "scum")
        tmp8 = P.tile([128, NE], F32, "stmp8")
        E16 = P.tile([128, NG], F32, "sE16")
        O16 = P.tile([128, NG], F32, "sO16")
        t16 = P.tile([128, NG], F32, "st16")
        B16 = P.tile([128, NG], F32, "sB16")
        fH = P.tile([128, NG, TPG], F32, "sfH")
        fW = P.tile([128, NG, 11], F32, "sfW")
        fW2 = P.tile([128, NG, 2], F32, "sfW2")

        def s1_set(i):
            d = {}
            d["junk"] = P.tile([128, D], F32, f"sjunk{i}")
            d["h32"] = P.tile([128, D], F32, f"sh32{i}")
            d["hT32"] = P.tile([128, 8, 128], F32, f"shT32{i}")
            for nm in ("ss", "ms", "rstd", "m1", "m2", "dd"):
                d[nm] = P.tile([128, 1], F32, f"s{nm}{i}")
            for nm in ("lg", "lg2", "mk1", "mk2", "rk", "rk2"):
                d[nm] = P.tile([128, NE], F32, f"s{nm}{i}")
            d["mk12"] = P.tile([128, NE], BF16, f"smk12{i}")
            d["rf"] = P.tile([128, 2], F32, f"srf{i}")
            d["banks"] = (PS[1], PS[2], PS[3], PS[4]) if i == 0 else (PS[5], PS[6], PS[7], PS[0])
            return d
        sets = [s1_set(0), s1_set(1)]
        P.dma("sp", rw, rw_d[0].rearrange("(k p) e -> p k e", p=128), rw)
        P.dma("sp", ebase, ebase_d, ebase)
        P.dma("sp", giota, giota_d, giota)
        P.dma("sp", cth, cth_d, cth)
        P.dma("sp", ctw, ctw_d, ctw)
        P.dma("sp", ctw2, ctw2_d, ctw2)
        P.tt(ustr, tri, ident, ALU.subtract)
        P.memset(runc, 0.0)

        def g_route(t):
            d = sets[t % 2]
            junk, h32, hT32 = d["junk"], d["h32"], d["hT32"]
            ss, ms, rstd, m1, m2, dd = d["ss"], d["ms"], d["rstd"], d["m1"], d["m2"], d["dd"]
            lg, lg2, mk1, mk2, rk, rk2, mk12, rf = d["lg"], d["lg2"], d["mk1"], d["mk2"], d["rk"], d["rk2"], d["mk12"], d["rf"]
            bt0, bt1, blg, brk = d["banks"]
            xt = xts[t % 2]
            hbf = hbfs[t % 2]
            P.dma("sp", xt, src_d[t * 128:(t + 1) * 128, :], xt); yield
            P.act(junk, xt, AF.Square, accum=ss); yield
            P.ts(ms, ss, 1.0 / D, EPS, ALU.mult, ALU.add); yield
            P.rsqrt(rstd, ms, nhalf[:, 0:1]); yield
            P.stt(h32, xt, rstd, modb[:, 0, :], ALU.mult, ALU.mult); yield
            P.tt(h32, h32, modb[:, 1, :], ALU.add); yield
            P.copy(hbf, h32, eng="act"); yield
            for hh, bk in ((0, bt0), (1, bt1)):
                P.pe([("tr", bk[:, g * 128:(g + 1) * 128],
                       h32[:, (hh * 4 + g) * 128:(hh * 4 + g + 1) * 128], ident32) for g in range(4)]); yield
                P.copy(hT32[:, hh * 4:hh * 4 + 4, :], bk.re("p (k t) -> p k t", k=4), eng="act"); yield
            P.pe([("mm", blg[:, 0:NE], hT32[:, k, :], rw[:, k, :], k == 0, k == 7) for k in range(8)]); yield
            P.copy(lg, blg[:, 0:NE]); yield
            P.reduce(m1, lg, op=ALU.max); yield
            P.ts(mk1, lg, m1, None, ALU.is_ge); yield
            P.stt(lg2, mk1, -1e30, lg, ALU.mult, ALU.add); yield
            P.reduce(m2, lg2, op=ALU.max); yield
            P.ts(mk2, lg2, m2, None, ALU.is_ge); yield
            P.tt(dd, m2, m1, ALU.subtract); yield
            P.act(dd, dd, AF.Exp); yield
            P.ts(dd, dd, 1.0, None, ALU.add); yield
            P.recip(gw[:, t, 0:1], dd); yield
            P.ts(gw[:, t, 1:2], gw[:, t, 0:1], -1.0, 1.0, ALU.mult, ALU.add); yield
            P.tt(mk12, mk1, mk2, ALU.add); yield
            P.pe([("mm", brk[:, 0:NE], ustr, mk12, True, True),
                  ("mm", brk[:, NE:2 * NE], ones, mk12, True, True)]); yield
            P.tt(rk, brk[:, 0:NE], runc, ALU.add)
            P.tt(runc, runc, brk[:, NE:2 * NE], ALU.add); yield
            P.tt(rk, rk, ebase, ALU.add); yield
            P.stt(rk2, rk, 1.0, mk1, ALU.mult, ALU.mult, accum=rf[:, 0:1]); yield
            P.stt(rk2, rk, 1.0, mk2, ALU.mult, ALU.mult, accum=rf[:, 1:2]); yield
            P.copy(ridx[:, t, :], rf); yield
            P.idma(hs_d, hbf, ridx[:, t, 0:1], hbf, True); yield
            P.idma(hs_d, hbf, ridx[:, t, 1:2], hbf, True); yield

        pend = []
        for t in range(nt_run):
            pend.append(g_route(t))
            for _ in range(20):
                for g in list(pend):
                    try:
                        next(g)
                    except StopIteration:
                        pend.remove(g)
        while pend:
            for g in list(pend):
                try:
                    next(g)
                except StopIteration:
                    pend.remove(g)

        P.memset(ng, 0.0)
        for jj in range(6):
            P.ts(tmp8, runc, float(jj * TB) + 0.5, None, ALU.is_gt)
            P.tt(ng, ng, tmp8, ALU.add)
        P.copy(cum, ng)
        for e in range(1, NE):
            P.tt(cum[:, e:e + 1], cum[:, e:e + 1], cum[:, e - 1:e], ALU.add)
        P.memset(E16, 0.0)
        P.copy(O16, giota)
        for e in range(NE):
            P.ts(t16, giota, cum[:, e:e + 1], None, ALU.is_ge)
            P.tt(E16, E16, t16, ALU.add)
            P.ts(t16, t16, ng[:, e:e + 1], None, ALU.mult)
            P.tt(O16, O16, t16, ALU.subtract)
        P.ts(E16, E16, float(NE - 1), None, ALU.min)
        P.ts(O16, O16, 5.0, 0.0, ALU.min, ALU.max)
        P.ts(B16, E16, float(ESTR), None, ALU.mult)
        P.stt(B16, O16, float(TB), B16, ALU.mult, ALU.add)
        P.tt(fH, B16.unsq(2).bc([128, NG, TPG]), cth[:, 0:TPG].unsq(1).bc([128, NG, TPG]), ALU.add)
        P.copy(iH, fH)
        P.ts(t16, E16, float(11 * 128), None, ALU.mult)
        P.tt(fW, t16.unsq(2).bc([128, NG, 11]), ctw.unsq(1).bc([128, NG, 11]), ALU.add)
        P.copy(iW, fW)
        P.ts(t16, E16, 256.0, None, ALU.mult)
        P.tt(fW2, t16.unsq(2).bc([128, NG, 2]), ctw2.unsq(1).bc([128, NG, 2]), ALU.add)
        P.copy(iW2, fW2)
        P.barrier()

        P.sb_off = mark
        hTs = [P.tile([128, 8, TB], BF16, f"shT{i}") for i in range(2)]
        actT = P.tile([128, NF, TB], BF16, "sactT")
        W2 = P.tile([128, NF, D], BF16, "sW2")
        NWB = 4
        W1g = [P.tile([128, 8, 256], BF16, f"sW1g{i}") for i in range(NWB)]
        W3g = [P.tile([128, 8, 256], BF16, f"sW3g{i}") for i in range(NWB)]
        sg = [P.tile([128, 512], BF16, f"ssg{i}") for i in range(2)]
        ngroups = NG if n_tiles_dbg is None else max(1, n_tiles_dbg // 2)
        gi = 0
        W2f = W2.re("p f n -> p (f n)")

        def slot_a(g, ti):
            hsl = hbfs[ti % 2]
            P.idma(hsl, hs_d, iH[:, g, ti:ti + 1], hsl, False)

        def slot_b(g, ti, bank):
            hsl = hbfs[ti % 2]
            tp = PS[bank].bitcast(BF16)
            P.pe([("tr", tp[:, j * 128:(j + 1) * 128], hsl[:, j * 128:(j + 1) * 128], ident) for j in range(8)])
            P.copy(hTs[g % 2][:, :, ti * 128:(ti + 1) * 128], tp.re("p (k t) -> p k t", k=8), eng="act")

        for g in range(ngroups):
            for half in range(2):
                P.idma(W2f[:, half * 11 * D:(half + 1) * 11 * D], w2L_d, iW2[:, g, half:half + 1], W2, False)
            hT = hTs[g % 2]
            if g == 0:
                for ti in range(TPG):
                    slot_a(0, ti)
                    slot_b(0, ti, ti % 2)
            for fg in range(NF // 2):
                wa, wb = W1g[gi % NWB], W3g[gi % NWB]
                gi += 1
                P.idma(wa.re("p k c -> p (k c)"), w1L_d, iW[:, g, fg:fg + 1], wa, False)
                P.idma(wb.re("p k c -> p (k c)"), w3L_d, iW[:, g, fg:fg + 1], wb, False)
                for fi in range(2):
                    f = fg * 2 + fi
                    for sub in range(2):
                        gp = PS[(2 * sub) % 4]
                        up = PS[(2 * sub + 1) % 4]
                        SW = TB // 2
                        tok = slice(sub * SW, (sub + 1) * SW)
                        P.pe([("mm", gp[:, 0:SW], wa[:, k, fi * 128:(fi + 1) * 128], hT[:, k, tok], k == 0, k == 7)
                              for k in range(8)])
                        P.pe([("mm", up[:, 0:SW], wb[:, k, fi * 128:(fi + 1) * 128], hT[:, k, tok], k == 0, k == 7)
                              for k in range(8)])
                        s_ = sg[sub]
                        P.act(s_[:, 0:SW], gp[:, 0:SW], AF.Silu)
                        P.tt(actT[:, f, tok], s_[:, 0:SW], up[:, 0:SW], ALU.mult)
                if g + 1 < ngroups:
                    if 1 <= fg <= TPG:
                        slot_b(g + 1, fg - 1, 7)
                    if fg < TPG:
                        slot_a(g + 1, fg)
            for ti in range(TPG):
                c0 = ti * 128
                pa, pb = PS[4 + 2 * (ti % 2)], PS[5 + 2 * (ti % 2)]
                items = []
                for f in range(NF):
                    l = actT[:, f, c0:c0 + 128]
                    items.append(("mm", pa, l, W2[:, f, 0:512], f == 0, f == NF - 1))
                    items.append(("mm", pb, l, W2[:, f, 512:1024], f == 0, f == NF - 1))
                P.pe(items)
                yb = ysb[ti % 2]
                P.copy(yb[:, 0:512], pa, eng="act")
                P.copy(yb[:, 512:1024], pb)
                P.idma(ys_d, yb, iH[:, g, ti:ti + 1], yb, True)
        P.barrier()

        def s4_loads(t):
            xt = xts[t % 2]
            y0, y1 = ysb[2 * (t % 2)], ysb[2 * (t % 2) + 1]
            P.dma("sp", xt, src_d[t * 128:(t + 1) * 128, :], xt)
            P.idma(y0, ys_d, ridx[:, t, 0:1], y0, False)
            P.idma(y1, ys_d, ridx[:, t, 1:2], y1, False)

        s4_loads(0)
        for t in range(nt_run):
            xt = xts[t % 2]
            y0, y1 = ysb[2 * (t % 2)], ysb[2 * (t % 2) + 1]
            if t + 1 < nt_run:
                s4_loads(t + 1)
            P.ts(y0, y0, gw[:, t, 0:1], None, ALU.mult)
            P.stt(y0, y1, gw[:, t, 1:2], y0, ALU.mult, ALU.add)
            P.tt(y0, y0, modb[:, 2, :], ALU.mult)
            P.tt(xt, y0, xt, ALU.add)
            P.dma("sp", dst_d[t * 128:(t + 1) * 128, :], xt, xt)
        P.barrier()
        P.sb_off = m

    phases = [
        ("mix0", lambda s, d: mixer(0, s, d)),
        ("ffn0", lambda s, d: ffn(0, s, d, [fw1L_d], [fw3L_d], [fw2L_d], False)),
        ("mix1", lambda s, d: mixer(1, s, d)),
        ("ffn1", (lambda s, d: moe_sparse(1, s, d)) if SPARSE else
         (lambda s, d: ffn(1, s, d, [moe_w1_d[0, e] for e in range(NE)],
                           [moe_w3_d[0, e] for e in range(NE)],
                           [moe_w2_d[0, e] for e in range(NE)], True))),
    ]
    if stop_after == "ffn1only":
        phases = phases[3:]
    elif stop_after is not None:
        phases = phases[:[p[0] for p in phases].index(stop_after) + 1]
    for i, (name, fn) in enumerate(phases):
        src = x_d if i == 0 else xs_d
        dst = y_d if i == len(phases) - 1 else xs_d
        fn(src, dst)
    P.barrier()
    P.finish()
    return nc, stack


def make_in_maps(inputs, cores):
    f = np.float32
    A = lambda a: np.ascontiguousarray(a)
    bc = lambda a: A(np.broadcast_to(a[:, None, :], (a.shape[0], 128, a.shape[1])))
    inv_freq = (1.0 / (np.float32(10000.0) ** (np.arange(0, 64, 2, dtype=f) / f(64)))).astype(f)
    shared = {
        "invf": A(np.broadcast_to(inv_freq[None, :], (128, 32))),
        "ident": np.eye(128, dtype=f),
        "tri": A(np.triu(np.ones((128, 128), dtype=f))),
        "ada_w": A(inputs["ada_w"]), "ada_b": A(inputs["ada_b"]),
        "nmix_b": bc(inputs["norm_mix"]), "nffn_b": bc(inputs["norm_ffn"]),
        "w_in": A(inputs["w_in"]),
        "sgn_b": bc(inputs["sgu_norm"]),
        "sguT": A(np.transpose(inputs["sgu_w"], (0, 3, 1, 2))),
        "sgb": A(np.repeat(np.transpose(inputs["sgu_b"], (0, 2, 1)), 64, axis=2)),
        "qln_b": bc(inputs["q_lat_norm"]), "kvln_b": bc(inputs["kv_lat_norm"]),
        "w_uq": A(inputs["w_uq"]), "w_ukv": A(inputs["w_ukv"]),
        "qn_b": bc(inputs["q_norm"]), "kn_b": bc(inputs["k_norm"]),
        "w_out": A(inputs["w_out"]),
        "fw1L": A(inputs["ffn_w1"][0].reshape(8, 128, 11, 256).transpose(2, 1, 0, 3).reshape(11 * 128, 2048)),
        "fw3L": A(inputs["ffn_w3"][0].reshape(8, 128, 11, 256).transpose(2, 1, 0, 3).reshape(11 * 128, 2048)),
        "fw2L": A(inputs["ffn_w2"][0].reshape(NF, 128, D).transpose(1, 0, 2).reshape(128, NF * D)),
        "router_w": A(inputs["router_w"]),
    }
    def lay13(w):
        return A(w[0].reshape(NE, 8, 128, 11, 256).transpose(0, 3, 2, 1, 4).reshape(NE * 11 * 128, 8 * 256))
    shared["w1L"] = lay13(inputs["moe_w1"])
    shared["w3L"] = lay13(inputs["moe_w3"])
    shared["w2L"] = A(inputs["moe_w2"][0].reshape(NE, 2, 11, 128, D).transpose(0, 3, 1, 2, 4).reshape(NE * 128 * 2, 11 * D))
    pp = np.arange(128, dtype=f)[:, None]
    shared["ebase"] = A(np.broadcast_to((np.arange(NE, dtype=f) * 4608)[None, :], (128, NE)))
    shared["giota"] = A(np.broadcast_to(np.arange(18, dtype=f)[None, :], (128, 18)))
    shared["ct_h"] = A(np.arange(8, dtype=f)[None, :] * 128 + pp)
    shared["ct_w"] = A(np.arange(11, dtype=f)[None, :] * 128 + pp)
    shared["ct_w2"] = A(np.arange(2, dtype=f)[None, :] + 2 * pp)
    maps = []
    for b in cores:
        mp = dict(shared)
        mp["x"] = A(inputs["x"][b])
        mp["cT"] = A(inputs["c"][b].reshape(8, 128).T)
        mp["pos"] = A(inputs["positions"][b].reshape(NT, 128).T.astype(np.int32))
        maps.append(mp)
    return maps


def kernel(**inputs):
    inputs = {k: np.asarray(v) for k, v in inputs.items()}
    nc, stack = build_program()
    with stack:
        in_maps = make_in_maps(inputs, list(range(8)))
        res = run_bass_kernel_spmd(nc, in_maps, core_ids=list(range(8)))
    return np.stack([np.asarray(r["y"], dtype=np.float32) for r in res.results], axis=0)
```

```python
import contextlib
import numpy as np
import concourse.bass as bass
import concourse.mybir as mybir
from concourse.bass_utils import run_bass_kernel_spmd

F32, BF16, I32 = mybir.dt.float32, mybir.dt.bfloat16, mybir.dt.int32
AF = mybir.ActivationFunctionType
ALU = mybir.AluOpType
AX = mybir.AxisListType

S = 4096
D = 1024
NT = S // 128
DFF = 2816
NF = DFF // 128
NE = 8
EPS = 1e-6
D_IN = 1472
ENGS = ("sp", "act", "dve", "pool", "pe")
_DT_SIZE = {F32: 4, BF16: 2, I32: 4}


class Buf:
    __slots__ = ("name", "w", "r", "dsem", "dcnt")

    def __init__(self, name):
        self.name = name
        self.w = None
        self.r = {}
        self.dsem = None
        self.dcnt = 0


class View:
    def __init__(self, ap, buf):
        self.ap = ap
        self.buf = buf

    def __getitem__(self, k):
        return View(self.ap[k], self.buf)

    def re(self, s, **kw):
        return View(self.ap.rearrange(s, **kw), self.buf)

    def bc(self, shape):
        return View(self.ap.to_broadcast(list(shape)), self.buf)

    def bitcast(self, dt):
        return View(self.ap.bitcast(dt), self.buf)

    def unsq(self, ax):
        return View(self.ap.unsqueeze(ax), self.buf)

    def on(self, buf):
        return View(self.ap, buf)


def _ap(v):
    return v.ap if isinstance(v, View) else v


class Prog:
    def __init__(self, nc, stack):
        self.nc = nc
        self.stack = stack
        self.q = {e: [] for e in ENGS}
        self.sem = {}
        self.cnt = {}
        self.waited = {e: {} for e in ENGS}
        self.nsem = 0
        for e in ENGS:
            self._new_sem(e)
        self.dma_events = {}
        self.sb_off = 16640
        self.sb_top = 229344
        self.ntile = 0

    def tile(self, shape, dt, name="t", nbuf=None):
        nbytes = int(np.prod(shape[1:])) * _DT_SIZE[dt]
        nbytes = (nbytes + 63) // 64 * 64
        off = self.sb_off
        self.sb_off += nbytes
        assert self.sb_off <= self.sb_top, f"SBUF overflow at {name}: {self.sb_off}"
        self.ntile += 1
        h = self.nc.alloc_sbuf_tensor_at(f"{name}_{self.ntile}", list(shape), dt, offset=off)
        return View(h.ap(), Buf(name))

    def psum(self, name):
        self.ntile += 1
        h = self.nc.alloc_psum_tensor(f"{name}_{self.ntile}", [128, 512], F32)
        return View(h.ap(), Buf(name))

    def _alloc_sem(self, name):
        self.nsem += 1
        return self.stack.enter_context(self.nc.semaphore(f"{name}_{self.nsem}"))

    def _new_sem(self, e):
        self.sem[e] = self._alloc_sem(f"s_{e}")
        self.cnt[e] = 0

    def wait(self, e, ev):
        sem, v = ev
        if self.waited[e].get(sem, 0) >= v:
            return
        self.waited[e][sem] = v
        self.q[e].append(lambda eng, sem=sem, v=v: eng.wait_ge(sem, v))

    def _deps(self, e, reads, writes):
        deps = {}

        def add(sem, v):
            if deps.get(sem, 0) < v:
                deps[sem] = v
        for b in reads:
            if b.w is not None:
                add(*b.w)
        for b in writes:
            if b.w is not None:
                add(*b.w)
            for sem, v in b.r.items():
                add(sem, v)
        for sem, v in deps.items():
            self.wait(e, (sem, v))

    def _record(self, ev, reads, writes):
        for b in reads:
            if b.r.get(ev[0], 0) < ev[1]:
                b.r[ev[0]] = ev[1]
        for b in writes:
            b.w = ev
            b.r = {}

    def emit(self, e, fn, reads=(), writes=()):
        reads = [b for b in reads if b is not None]
        writes = [b for b in writes if b is not None]
        self._deps(e, reads, writes)
        if self.cnt[e] >= 30000:
            self._new_sem(e)
        self.cnt[e] += 1
        ev = (self.sem[e], self.cnt[e])
        self.q[e].append(lambda eng, sem=ev[0]: fn(eng).then_inc(sem, 1))
        self._record(ev, reads, writes)
        return ev

    def emit_group(self, e, fns, reads, writes):
        self._deps(e, reads, writes)
        if self.cnt[e] >= 30000:
            self._new_sem(e)
        self.cnt[e] += 1
        ev = (self.sem[e], self.cnt[e])
        for fn in fns[:-1]:
            self.q[e].append(lambda eng, fn=fn: fn(eng))
        self.q[e].append(lambda eng, sem=ev[0], fn=fns[-1]: fn(eng).then_inc(sem, 1))
        self._record(ev, reads, writes)
        return ev

    def dma(self, e, out, in_, sb):
        b = sb.buf
        reads = [in_.buf] if isinstance(in_, View) else []
        writes = [out.buf] if isinstance(out, View) else []
        self._deps(e, reads, writes)
        if b.dsem is None:
            b.dsem = self._alloc_sem("d_" + b.name)
        b.dcnt += 16
        ev = (b.dsem, b.dcnt)
        o, i = _ap(out), _ap(in_)
        self.q[e].append(lambda eng, o=o, i=i, sem=ev[0]: eng.dma_start(out=o, in_=i).then_inc(sem, 16))
        self._record(ev, reads, writes)
        self.dma_events[ev[0]] = ev[1]
        return ev

    def idma(self, out, in_, idx, sb, scatter):
        e = "pool"
        b = sb.buf
        reads = [idx.buf] + ([in_.buf] if isinstance(in_, View) else [])
        writes = [out.buf] if isinstance(out, View) else []
        self._deps(e, reads, writes)
        if b.dsem is None:
            b.dsem = self._alloc_sem("d_" + b.name)
        b.dcnt += 16
        ev = (b.dsem, b.dcnt)
        o, i, ix = _ap(out), _ap(in_), idx.ap

        def th(eng, o=o, i=i, ix=ix, sem=ev[0], scatter=scatter):
            off = bass.IndirectOffsetOnAxis(ap=ix, axis=0)
            if scatter:
                ins = eng.indirect_dma_start(out=o, out_offset=off, in_=i, in_offset=None)
            else:
                ins = eng.indirect_dma_start(out=o, out_offset=None, in_=i, in_offset=off)
            ins.then_inc(sem, 16)
        self.q[e].append(th)
        self._record(ev, reads, writes)
        self.dma_events[ev[0]] = ev[1]
        return ev

    def barrier(self):
        evs = [(self.sem[e], self.cnt[e]) for e in ENGS if self.cnt[e] > 0]
        evs += list(self.dma_events.items())
        for e in ENGS:
            for ev in evs:
                self.wait(e, ev)
        self.dma_events = {}

    def act(self, out, in_, func, scale=1.0, bias=0.0, accum=None):
        reads = [in_.buf] + [v.buf for v in (scale, bias) if isinstance(v, View)]
        writes = [out.buf] + ([accum.buf] if accum is not None else [])
        kw = dict(out=out.ap, in_=in_.ap, func=func, scale=_ap(scale), bias=_ap(bias))
        if accum is not None:
            kw["accum_out"] = accum.ap
        return self.emit("act", lambda e: e.activation(**kw), reads, writes)

    def tt(self, out, a, b, op, eng="dve"):
        return self.emit(eng, lambda e: e.tensor_tensor(out=out.ap, in0=a.ap, in1=b.ap, op=op),
                         [a.buf, b.buf], [out.buf])

    def ts(self, out, a, s1, s2, op0, op1=None, eng="dve", accum=None):
        reads = [a.buf] + [v.buf for v in (s1, s2) if isinstance(v, View)]
        writes = [out.buf] + ([accum.buf] if accum is not None else [])
        kw = dict(out=out.ap, in0=a.ap, scalar1=_ap(s1), scalar2=_ap(s2), op0=op0)
        if op1 is not None:
            kw["op1"] = op1
        if accum is not None:
            kw["accum_out"] = accum.ap
        return self.emit(eng, lambda e: e.tensor_scalar(**kw), reads, writes)

    def stt(self, out, a, s, b, op0, op1, accum=None):
        reads = [a.buf, b.buf] + ([s.buf] if isinstance(s, View) else [])
        writes = [out.buf] + ([accum.buf] if accum is not None else [])
        kw = dict(out=out.ap, in0=a.ap, scalar=_ap(s), in1=b.ap, op0=op0, op1=op1)
        if accum is not None:
            kw["accum_out"] = accum.ap
        return self.emit("dve", lambda e: e.scalar_tensor_tensor(**kw), reads, writes)

    def copy(self, out, a, eng="dve"):
        if eng == "act":
            return self.emit("act", lambda e: e.copy(out=out.ap, in_=a.ap), [a.buf], [out.buf])
        return self.emit(eng, lambda e: e.tensor_copy(out=out.ap, in_=a.ap), [a.buf], [out.buf])

    def memset(self, out, val, eng="pool"):
        return self.emit(eng, lambda e: e.memset(out.ap, val), [], [out.buf])

    def reduce(self, out, a, op=ALU.add):
        return self.emit("dve", lambda e: e.tensor_reduce(out=out.ap, in_=a.ap, axis=AX.X, op=op),
                         [a.buf], [out.buf])

    def recip(self, out, a):
        return self.emit("dve", lambda e: e.reciprocal(out=out.ap, in_=a.ap), [a.buf], [out.buf])

    def rsqrt(self, out, a, nh):
        return self.emit("pool", lambda e: e.tensor_tensor(out=out.ap, in0=a.ap, in1=nh.ap, op=ALU.pow),
                         [a.buf, nh.buf], [out.buf])

    def pe(self, items):
        fns, reads, writes = [], [], []
        for it in items:
            if it[0] == "mm":
                _, out, l, r, st, sp = it
                fns.append(lambda e, out=out, l=l, r=r, st=st, sp=sp:
                           e.matmul(out.ap, l.ap, r.ap, start=st, stop=sp))
                reads += [l.buf, r.buf]
                writes.append(out.buf)
            else:
                _, out, a, idn = it
                fns.append(lambda e, out=out, a=a, idn=idn: e.transpose(out.ap, a.ap, idn.ap))
                reads += [a.buf, idn.buf]
                writes.append(out.buf)
        reads = list({id(b): b for b in reads}.values())
        writes = list({id(b): b for b in writes}.values())
        return self.emit_group("pe", fns, reads, writes)

    def finish(self):
        nc = self.nc
        q = self.q
        with nc.Block() as block:
            @block.sync
            def _(eng):
                for th in q["sp"]:
                    th(eng)

            @block.scalar
            def _(eng):
                for th in q["act"]:
                    th(eng)

            @block.vector
            def _(eng):
                for th in q["dve"]:
                    th(eng)

            @block.gpsimd
            def _(eng):
                for th in q["pool"]:
                    th(eng)

            @block.tensor
            def _(eng):
                for th in q["pe"]:
                    th(eng)


def build_program(stop_after=None, n_tiles_dbg=None):
    nc = bass.Bass("TRN2", target_bir_lowering=False)
    stack = contextlib.ExitStack()
    P = Prog(nc, stack)

    def din(name, shape, dt=F32):
        return nc.dram_tensor(name, list(shape), dt, kind="ExternalInput").ap()

    x_d = din("x", [S, D])
    cT_d = din("cT", [128, 8])
    pos_d = din("pos", [128, NT], I32)
    invf_d = din("invf", [128, 32])
    ident_d = din("ident", [128, 128])
    tri_d = din("tri", [128, 128])
    ada_w_d = din("ada_w", [2, D, 6 * D])
    ada_b_d = din("ada_b", [2, 6 * D])
    nmix_d = din("nmix_b", [2, 128, D])
    nffn_d = din("nffn_b", [2, 128, D])
    w_in_d = din("w_in", [2, D, D_IN])
    sgn_d = din("sgn_b", [2, 128, 512])
    sguT_d = din("sguT", [2, 128, 8, 128])
    sgb_d = din("sgb", [2, 128, 512])
    qln_d = din("qln_b", [2, 128, 256])
    kvln_d = din("kvln_b", [2, 128, 128])
    w_uq_d = din("w_uq", [2, 256, 768])
    w_ukv_d = din("w_ukv", [2, 128, 1024])
    qn_d = din("qn_b", [2, 128, 192])
    kn_d = din("kn_b", [2, 128, 192])
    w_out_d = din("w_out", [2, D, D])
    fw1L_d = din("fw1L", [11 * 128, 2048])
    fw3L_d = din("fw3L", [11 * 128, 2048])
    fw2L_d = din("fw2L", [128, NF * D])
    rw_d = din("router_w", [1, D, NE])
    SPARSE = True
    if SPARSE:
        w1L_d = din("w1L", [NE * 11 * 128, 2048])
        w3L_d = din("w3L", [NE * 11 * 128, 2048])
        w2L_d = din("w2L", [NE * 128 * 2, 11 * D])
        ebase_d = din("ebase", [128, NE])
        giota_d = din("giota", [128, 18])
        cth_d = din("ct_h", [128, 8])
        ctw_d = din("ct_w", [128, 11])
        ctw2_d = din("ct_w2", [128, 2])
        hs_d = nc.dram_tensor("hs", [NE * 4608, D], BF16, kind="Internal").ap()
        ys_d = nc.dram_tensor("ys", [NE * 4608, D], F32, kind="Internal").ap()
    else:
        moe_w1_d = din("moe_w1", [1, NE, D, DFF])
        moe_w3_d = din("moe_w3", [1, NE, D, DFF])
        moe_w2_d = din("moe_w2", [1, NE, DFF, D])
    y_d = nc.dram_tensor("y", [S, D], F32, kind="ExternalOutput").ap()
    xs_d = nc.dram_tensor("xs", [S, D], F32, kind="Internal").ap()

    nt_run = NT if n_tiles_dbg is None else n_tiles_dbg

    PS = [P.psum(f"ps{i}") for i in range(8)]

    ident = P.tile([128, 128], BF16, "ident")
    tri = P.tile([128, 128], BF16, "tri")
    ones = P.tile([128, 128], BF16, "ones")
    ident32 = P.tile([128, 128], F32, "ident32")
    nhalf = P.tile([128, 8], F32, "nhalf")
    cosT = P.tile([128, NT, 32], BF16, "cos")
    sinT = P.tile([128, NT, 32], BF16, "sin")
    cact = P.tile([128, 8], F32, "cact")
    ones1 = P.tile([1, 128], F32, "ones1")
    modb = P.tile([128, 3, D], BF16, "modb")
    persist_mark = P.sb_off

    P.dma("pool", ident, ident_d, ident)
    P.dma("pool", tri, tri_d, tri)
    P.dma("sp", ident32, ident_d, ident32)
    P.memset(ones, 1.0)
    P.memset(nhalf, -0.5)
    P.memset(ones1, 1.0)
    m0 = P.sb_off
    cT = P.tile([128, 8], F32, "cT")
    posi = P.tile([128, NT], I32, "posi")
    posf = P.tile([128, NT], F32, "posf")
    invf = P.tile([128, 32], F32, "invf")
    ang = P.tile([128, NT, 32], F32, "ang")
    ang2 = P.tile([128, NT, 32], F32, "ang2")
    P.dma("sp", cT, cT_d, cT)
    P.dma("sp", posi, pos_d, posi)
    P.dma("sp", invf, invf_d, invf)
    P.act(cact, cT, AF.Silu)
    P.copy(posf, posi)
    P.tt(ang, posf.unsq(2).bc([128, NT, 32]), invf.unsq(1).bc([128, NT, 32]), ALU.mult)
    angi = P.tile([128, NT, 32], I32, "angi")
    angf = P.tile([128, NT, 32], F32, "angf")
    msk = P.tile([128, NT, 32], F32, "msk")

    def sin_of(dst, shift):
        P.ts(ang2, ang, float(1.0 / (2.0 * np.pi)), float(shift), ALU.mult, ALU.add)
        P.copy(angi, ang2)
        P.copy(angf, angi)
        P.tt(ang2, ang2, angf, ALU.subtract)
        P.ts(msk, ang2, 0.5, None, ALU.is_gt)
        P.tt(ang2, ang2, msk, ALU.subtract)
        P.ts(msk, ang2, -0.5, None, ALU.is_lt)
        P.tt(ang2, ang2, msk, ALU.add)
        P.act(dst, ang2, AF.Sin, scale=6.283185)

    sin_of(sinT, 0.0)
    sin_of(cosT, 0.25)
    P.barrier()
    P.sb_off = m0

    def compute_mod(layer, sub, norm_d):
        m = P.sb_off
        wbuf = [P.tile([128, 3 * D], F32, f"adaw{i}") for i in range(2)]
        crep = P.tile([128, 8, 128], F32, "crep")
        for k in range(8):
            P.copy(crep[:, k, :], cact[:, k:k + 1].bc([128, 128]))
        brow = P.tile([1, 3 * D], F32, "brow")
        gb = P.tile([128, D], F32, "gb")
        t1 = P.tile([128, D], F32, "t1")
        c0 = sub * 3 * D
        P.dma("sp", brow, ada_b_d[layer:layer + 1, c0:c0 + 3 * D], brow)
        P.dma("sp", gb, norm_d[layer], gb)
        for k in range(8):
            wb = wbuf[k % 2]
            P.dma("sp", wb, ada_w_d[layer, k * 128:(k + 1) * 128, c0:c0 + 3 * D], wb)
            for j in range(6):
                P.pe([("mm", PS[j], crep[:, k, :], wb[:, j * 512:(j + 1) * 512], k == 0, False)])
        for j in range(6):
            P.pe([("mm", PS[j], ones1, brow[:, j * 512:(j + 1) * 512], False, True)])
        for hh in range(2):
            P.stt(t1[:, hh * 512:(hh + 1) * 512], PS[2 + hh], 1.0, gb[:, hh * 512:(hh + 1) * 512],
                  ALU.add, ALU.mult)
            P.copy(modb[:, 1, hh * 512:(hh + 1) * 512], PS[0 + hh], eng="act")
            P.copy(modb[:, 2, hh * 512:(hh + 1) * 512], PS[4 + hh], eng="act")
        P.copy(modb[:, 0, :], t1)
        P.barrier()
        P.sb_off = m

    def norm_tile(xt, hT_dst, tmp, h32=None):
        junk, ss, ms, rstd, hbf, tpsum = tmp
        P.act(junk, xt, AF.Square, accum=ss)
        P.ts(ms, ss, 1.0 / D, EPS, ALU.mult, ALU.add)
        P.rsqrt(rstd, ms, nhalf[:, 0:1])
        hdst = h32 if h32 is not None else junk
        P.stt(hdst, xt, rstd, modb[:, 0, :], ALU.mult, ALU.mult)
        if h32 is not None:
            P.tt(h32, h32, modb[:, 1, :], ALU.add)
            P.copy(hbf, h32, eng="act")
        else:
            P.tt(hbf, junk, modb[:, 1, :], ALU.add)
        tp = tpsum.bitcast(BF16)
        P.pe([("tr", tp[:, j * 128:(j + 1) * 128], hbf[:, j * 128:(j + 1) * 128], ident) for j in range(8)])
        P.copy(hT_dst, tp.re("p (k t) -> p k t", k=8), eng="act")

    def mixer(layer, src_d, dst_d):
        m = P.sb_off
        Win = P.tile([128, 8, D_IN], BF16, "Win")
        Wuq = P.tile([128, 2, 768], BF16, "Wuq")
        Wukv = P.tile([128, 1024], BF16, "Wukv")
        Wout = P.tile([128, 8, D], BF16, "Wout")
        WcT = P.tile([128, 8, 128], BF16, "WcT")
        sgn = P.tile([128, 512], BF16, "sgn")
        sgb = P.tile([128, 512], BF16, "sgb")
        qln = P.tile([128, 256], F32, "qln")
        kvln = P.tile([128, 128], F32, "kvln")
        qn = P.tile([128, 192], F32, "qn")
        kn = P.tile([128, 192], F32, "kn")
        GQ = P.tile([128, 128], F32, "GQ")
        P.dma("pool", Win, w_in_d[layer].rearrange("(k p) n -> p k n", p=128), Win)
        P.dma("pool", Wuq, w_uq_d[layer].rearrange("(k p) n -> p k n", p=128), Wuq)
        P.dma("pool", Wukv, w_ukv_d[layer], Wukv)
        P.dma("pool", Wout, w_out_d[layer].rearrange("(k p) n -> p k n", p=128), Wout)
        P.dma("pool", WcT, sguT_d[layer], WcT)
        P.dma("pool", sgn, sgn_d[layer], sgn)
        P.dma("pool", sgb, sgb_d[layer], sgb)
        P.dma("sp", qln, qln_d[layer], qln)
        P.dma("sp", kvln, kvln_d[layer], kvln)
        P.dma("sp", qn, qn_d[layer], qn)
        P.dma("sp", kn, kn_d[layer], kn)
        compute_mod(layer, 0, nmix_d)
        P.tt(WcT, WcT, tri.unsq(1).bc([128, 8, 128]), ALU.mult)
        P.tt(Wout, Wout, modb[:, 2, :].unsq(1).bc([128, 8, D]), ALU.mult)
        P.tt(GQ, qn[:, 0:128], kn[:, 0:128], ALU.mult)
        KTt = P.tile([128, 4, S], BF16, "KT")
        RTt = P.tile([64, S], BF16, "RT")
        Vt = P.tile([128, NT, 512], BF16, "V")
        rstdk = P.tile([128, NT, 4], F32, "rstdk")
        kbufs = [Buf(f"kblk{j}") for j in range(8)]
        hT = P.tile([128, 8, 512], BF16, "hT")
        QTn = P.tile([128, 4, 512], BF16, "QTn")
        QTr = P.tile([64, 4, 512], BF16, "QTr")
        catT = P.tile([128, 8, 512], BF16, "catT")
        xts = [P.tile([128, D], F32, f"xt{i}") for i in range(2)]
        junk = P.tile([128, D], F32, "junk")
        hbf = P.tile([128, D], BF16, "hbf")
        ss = P.tile([128, 1], F32, "ss")
        ms = P.tile([128, 1], F32, "ms")
        rstd = P.tile([128, 1], F32, "rstd")
        gu = P.tile([128, 512], BF16, "gu")
        gv = P.tile([128, 512], F32, "gv")
        st6 = P.tile([128, 6], F32, "st6")
        mv = P.tile([128, 2], F32, "mv")
        rv = P.tile([128, 1], F32, "rv")
        vtmp = P.tile([128, 512], F32, "vtmp")
        vb = P.tile([128, 512], BF16, "vb")
        stmp = vtmp
        ab = P.tile([128, 512], BF16, "ab")
        lat = P.tile([128, 448], F32, "lat")
        sl = P.tile([128, 4], F32, "sl")
        latn = P.tile([128, 384], BF16, "latn")
        latT = P.tile([128, 3, 128], BF16, "latT")
        qf = P.tile([128, 768], F32, "qf")
        sq = P.tile([128, 768], F32, "sq")
        ssq = P.tile([128, 4], F32, "ssq")
        rq = P.tile([128, 4], F32, "rq")
        qnb = P.tile([128, 4, 128], BF16, "qnb")
        qrb = P.tile([128, 4, 64], BF16, "qrb")
        r1 = P.tile([128, 4, 64], F32, "r1")
        r2 = P.tile([128, 4, 64], F32, "r2")
        r3 = P.tile([128, 4, 64], F32, "r3")
        knb = P.tile([128, 4, 128], BF16, "knb")
        ssk = P.tile([128, 4], F32, "ssk")
        sskr = P.tile([128, 1], F32, "sskr")
        krb = P.tile([128, 64], BF16, "krb")
        k1 = P.tile([128, 64], F32, "k1")
        k2 = P.tile([128, 64], F32, "k2")
        k3 = P.tile([128, 64], F32, "k3")
        Es = [P.tile([128, 512], BF16, f"E{i}") for i in range(2)]
        rec = gv
        xr = [P.tile([128, D], F32, f"xr{i}") for i in range(2)]
        ytmp = junk

        nblk = (nt_run + 3) // 4
        for j in range(nblk):
            kb = kbufs[j]
            def g_prelude(t, ti):
                X = (PS[1], PS[2], PS[3]) if ti % 2 == 0 else (PS[4], PS[5], PS[6])
                c0 = ti * 128
                xt = xts[t % 2]
                P.dma("sp", xt, src_d[t * 128:(t + 1) * 128, :], xt); yield
                P.act(junk, xt, AF.Square, accum=ss); yield
                P.ts(ms, ss, 1.0 / D, EPS, ALU.mult, ALU.add); yield
                P.rsqrt(rstd, ms, nhalf[:, 0:1]); yield
                P.stt(junk, xt, rstd, modb[:, 0, :], ALU.mult, ALU.mult); yield
                P.tt(hbf, junk, modb[:, 1, :], ALU.add); yield
                tp = X[0].bitcast(BF16)
                P.pe([("tr", tp[:, jj * 128:(jj + 1) * 128], hbf[:, jj * 128:(jj + 1) * 128], ident) for jj in range(8)]); yield
                P.copy(hT[:, :, c0:c0 + 128], tp.re("p (k t) -> p k t", k=8), eng="act"); yield
                P.pe([("mm", X[2][:, 0:448], hT[:, k, c0:c0 + 128], Win[:, k, 1024:1472], k == 0, k == 7) for k in range(8)]); yield
                P.pe([("mm", X[1], hT[:, k, c0:c0 + 128], Win[:, k, 512:1024], k == 0, k == 7) for k in range(8)]); yield
                P.pe([("mm", X[0], hT[:, k, c0:c0 + 128], Win[:, k, 0:512], k == 0, k == 7) for k in range(8)]); yield

            for ti in range(4):
                t = 4 * j + ti
                c0 = ti * 128
                X = (PS[1], PS[2], PS[3]) if ti % 2 == 0 else (PS[4], PS[5], PS[6])
                if ti == 0:
                    for _ in g_prelude(t, ti):
                        pass
                sqk = Es[0].bitcast(F32)
                state = {"lat": False}

                def g_sgu(c0=c0):
                    P.act(gu, X[0], AF.Gelu_apprx_tanh); yield
                    P.act(gv, X[1], AF.Gelu_apprx_tanh); yield
                    P.emit("dve", lambda e: e.bn_stats(out=st6.ap, in_=gv.ap), [gv.buf], [st6.buf]); yield
                    P.emit("dve", lambda e: e.bn_aggr(out=mv.ap, in_=st6.ap), [st6.buf], [mv.buf]); yield
                    P.ts(rv, mv[:, 1:2], EPS, None, ALU.add); yield
                    P.rsqrt(rv, rv, nhalf[:, 0:1]); yield
                    P.ts(vtmp, gv, mv[:, 0:1], rv, ALU.subtract, ALU.mult); yield
                    P.tt(vb, vtmp, sgn, ALU.mult); yield
                    P.pe([("mm", PS[0][:, g * 64:(g + 1) * 64], WcT[:, g, :], vb[:, g * 64:(g + 1) * 64], True, True)
                          for g in range(8)]); yield
                    P.tt(stmp, PS[0], sgb, ALU.add); yield
                    P.tt(ab, stmp, gu, ALU.mult); yield
                    tp = PS[0].bitcast(BF16)
                    P.pe([("tr", tp[:, g * 128:(g + 1) * 128], ab[:, g * 128:(g + 1) * 128], ident) for g in range(4)]); yield
                    P.copy(catT[:, 0:4, c0:c0 + 128], tp[:, 0:512].re("p (k t) -> p k t", k=4), eng="act"); yield

                def g_latq(c0=c0, t=t):
                    P.copy(lat, X[2][:, 0:448], eng="act"); yield
                    P.stt(sq[:, 0:256], lat[:, 0:256], 1.0, lat[:, 0:256], ALU.mult, ALU.mult, accum=sl[:, 0:1]); yield
                    P.stt(sq[:, 256:384], lat[:, 256:384], 1.0, lat[:, 256:384], ALU.mult, ALU.mult, accum=sl[:, 1:2]); yield
                    P.stt(sq[:, 384:448], lat[:, 384:448], 1.0, lat[:, 384:448], ALU.mult, ALU.mult, accum=sskr); yield
                    P.ts(sl[:, 2:3], sl[:, 0:1], 1.0 / 256, EPS, ALU.mult, ALU.add); yield
                    P.ts(sl[:, 3:4], sl[:, 1:2], 1.0 / 128, EPS, ALU.mult, ALU.add); yield
                    P.rsqrt(sl[:, 2:4], sl[:, 2:4], nhalf[:, 0:2]); yield
                    P.stt(latn[:, 0:256], lat[:, 0:256], sl[:, 2:3], qln, ALU.mult, ALU.mult); yield
                    P.stt(latn[:, 256:384], lat[:, 256:384], sl[:, 3:4], kvln, ALU.mult, ALU.mult); yield
                    tp = X[2].bitcast(BF16)
                    P.pe([("tr", tp[:, g * 128:(g + 1) * 128], latn[:, g * 128:(g + 1) * 128], ident) for g in range(3)]); yield
                    P.copy(latT, tp[:, 0:384].re("p (k t) -> p k t", k=3), eng="act"); yield
                    P.pe([("mm", X[2], latT[:, 2, :], Wukv[:, 0:512], True, True),
                          ("mm", PS[7], latT[:, 2, :], Wukv[:, 512:1024], True, True)])
                    state["lat"] = True
                    yield
                    P.pe([("mm", X[0], latT[:, 0, :], Wuq[:, 0, 0:512], True, False),
                          ("mm", X[0], latT[:, 1, :], Wuq[:, 1, 0:512], False, True),
                          ("mm", X[1][:, 0:256], latT[:, 0, :], Wuq[:, 0, 512:768], True, False),
                          ("mm", X[1][:, 0:256], latT[:, 1, :], Wuq[:, 1, 512:768], False, True)]); yield
                    P.copy(qf[:, 0:512], X[0], eng="act"); yield
                    P.copy(qf[:, 512:768], X[1][:, 0:256], eng="act"); yield
                    P.tt(sq, qf, qf, ALU.mult); yield
                    P.reduce(ssq, sq.re("p (h d) -> p h d", h=4)); yield
                    P.ts(ssq, ssq, 1.0 / 192, EPS, ALU.mult, ALU.add); yield
                    P.rsqrt(rq, ssq, nhalf[:, 0:4]); yield
                    q3 = qf.re("p (h d) -> p h d", h=4)
                    sq3 = sq.re("p (h d) -> p h d", h=4)
                    P.tt(sq3[:, :, 0:128], q3[:, :, 0:128], GQ.unsq(1).bc([128, 4, 128]), ALU.mult); yield
                    P.tt(qnb, sq3[:, :, 0:128], rq.unsq(2).bc([128, 4, 128]), ALU.mult); yield
                    tp = X[0].bitcast(BF16)
                    P.pe([("tr", tp[:, h * 128:(h + 1) * 128], qnb[:, h, :], ident) for h in range(4)]); yield
                    P.copy(QTn[:, :, c0:c0 + 128], tp[:, 0:512].re("p (k t) -> p k t", k=4), eng="act"); yield
                    cs = cosT[:, t, :].unsq(1).bc([128, 4, 32])
                    sn = sinT[:, t, :].unsq(1).bc([128, 4, 32])
                    P.tt(r1, q3[:, :, 128:192], qn[:, 128:192].unsq(1).bc([128, 4, 64]), ALU.mult); yield
                    P.tt(r2[:, :, 0:32], r1[:, :, 0:32], cs, ALU.mult); yield
                    P.tt(r2[:, :, 32:64], r1[:, :, 32:64], cs, ALU.mult); yield
                    P.tt(r3[:, :, 0:32], r1[:, :, 32:64], sn, ALU.mult); yield
                    P.tt(r3[:, :, 32:64], r1[:, :, 0:32], sn, ALU.mult); yield
                    P.tt(r2[:, :, 0:32], r2[:, :, 0:32], r3[:, :, 0:32], ALU.subtract); yield
                    P.tt(r2[:, :, 32:64], r2[:, :, 32:64], r3[:, :, 32:64], ALU.add); yield
                    P.tt(qrb, r2, rq.unsq(2).bc([128, 4, 64]), ALU.mult); yield
                    tp = X[1].bitcast(BF16)
                    P.pe([("tr", tp[0:64, h * 128:(h + 1) * 128], qrb[:, h, :], ident) for h in range(4)]); yield
                    P.copy(QTr[:, :, c0:c0 + 128], tp[0:64, 0:512].re("p (k t) -> p k t", k=4), eng="act"); yield

                def g_kv(t=t, kb=kb):
                    while not state["lat"]:
                        yield
                    for half in range(2):
                        kv3 = (X[2] if half == 0 else PS[7]).re("p (h c) -> p h c", h=2)
                        P.copy(Vt[:, t, half * 256:(half + 1) * 256].re("p (h d) -> p h d", h=2).on(kb),
                               kv3[:, :, 128:256], eng="act"); yield
                        P.copy(knb[:, 2 * half:2 * half + 2, :], kv3[:, :, 0:128], eng="act"); yield
                        P.tt(sqk.re("p (h d) -> p h d", h=2), kv3[:, :, 0:128], knb[:, 2 * half:2 * half + 2, :],
                             ALU.mult); yield
                        P.reduce(ssk[:, 2 * half:2 * half + 2], sqk.re("p (h d) -> p h d", h=2)); yield
                    P.ts(ssk, ssk, sskr, 1.0 / 192, ALU.add, ALU.mult); yield
                    P.ts(ssk, ssk, EPS, None, ALU.add); yield
                    P.rsqrt(ssk, ssk, nhalf[:, 0:4]); yield
                    P.ts(rstdk[:, t, :].on(kb), ssk, float(192 ** -0.5), None, ALU.mult); yield
                    tp = PS[7].bitcast(BF16)
                    P.pe([("tr", tp[:, h * 128:(h + 1) * 128], knb[:, h, :], ident) for h in range(4)]); yield
                    P.copy(KTt[:, :, t * 128:(t + 1) * 128].on(kb), tp[:, 0:512].re("p (k t) -> p k t", k=4), eng="act"); yield
                    cs1 = cosT[:, t, :]
                    sn1 = sinT[:, t, :]
                    P.tt(k1, lat[:, 384:448], kn[:, 128:192], ALU.mult, eng="pool"); yield
                    P.tt(k2[:, 0:32], k1[:, 0:32], cs1, ALU.mult, eng="pool"); yield
                    P.tt(k2[:, 32:64], k1[:, 32:64], cs1, ALU.mult, eng="pool"); yield
                    P.tt(k3[:, 0:32], k1[:, 32:64], sn1, ALU.mult, eng="pool"); yield
                    P.tt(k3[:, 32:64], k1[:, 0:32], sn1, ALU.mult, eng="pool"); yield
                    P.tt(krb[:, 0:32], k2[:, 0:32], k3[:, 0:32], ALU.subtract, eng="pool"); yield
                    P.tt(krb[:, 32:64], k2[:, 32:64], k3[:, 32:64], ALU.add, eng="pool"); yield
                    tp = PS[7].bitcast(BF16)
                    P.pe([("tr", tp[0:64, 0:128], krb, ident)]); yield
                    P.copy(RTt[:, t * 128:(t + 1) * 128].on(kb), tp[0:64, 0:128], eng="act"); yield

                gens = [g_sgu(), g_latq(), g_kv()]
                if ti < 3:
                    gens.insert(0, g_prelude(t + 1, ti + 1))
                while gens:
                    for g in list(gens):
                        try:
                            next(g)
                        except StopIteration:
                            gens.remove(g)

            nkt = 4 * (j + 1)
            units = [(h, kt) for h in range(4) for kt in range(nkt)]
            Sb = [PS[0], PS[1]]

            def emit_S(i):
                h, kt = units[i]
                kbk = kbufs[kt // 4]
                qoff = max(0, kt - 4 * j) * 128
                sb = Sb[i % 2]
                P.pe([("mm", sb[:, qoff:512], KTt[:, h, kt * 128:(kt + 1) * 128].on(kbk), QTn[:, h, qoff:512], True, False),
                      ("mm", sb[:, qoff:512], RTt[:, kt * 128:(kt + 1) * 128].on(kbk), QTr[:, h, qoff:512], False, True)])

            emit_S(0)
            for i, (h, kt) in enumerate(units):
                if i + 1 < len(units):
                    emit_S(i + 1)
                kbk = kbufs[kt // 4]
                qoff = max(0, kt - 4 * j) * 128
                sb = Sb[i % 2]
                Ob = PS[2 + (h % 2)]
                Lb = PS[4 + (h % 2)]
                E = Es[i % 2]
                P.act(E[:, qoff:512], sb[:, qoff:512], AF.Exp, scale=rstdk[:, kt, h:h + 1].on(kbk))
                if kt >= 4 * j:
                    P.tt(E[:, qoff:qoff + 128], E[:, qoff:qoff + 128], tri, ALU.mult, eng="pool")
                P.pe([("mm", Ob[:, qoff:512], Vt[:, kt, h * 128:(h + 1) * 128].on(kbk), E[:, qoff:512], kt == 0, kt == nkt - 1),
                      ("mm", Lb[:, qoff:512], ones, E[:, qoff:512], kt == 0, kt == nkt - 1)])
                if kt == nkt - 1:
                    P.recip(rec, Lb)
                    P.tt(catT[:, 4 + h, :], Ob, rec, ALU.mult)

            P.dma("sp", xr[(4 * j) % 2], src_d[4 * j * 128:(4 * j + 1) * 128, :], xr[(4 * j) % 2])
            for ti in range(4):
                t = 4 * j + ti
                c0 = ti * 128
                x2 = xr[t % 2]
                if ti < 3:
                    P.dma("sp", xr[(t + 1) % 2], src_d[(t + 1) * 128:(t + 2) * 128, :], xr[(t + 1) % 2])
                items = []
                pa, pb = [(PS[6], PS[7]), (PS[0], PS[1]), (PS[2], PS[3]), (PS[4], PS[5])][ti]
                for k in range(8):
                    l = catT[:, k, c0:c0 + 128]
                    items.append(("mm", pa, l, Wout[:, k, 0:512], k == 0, k == 7))
                    items.append(("mm", pb, l, Wout[:, k, 512:1024], k == 0, k == 7))
                P.pe(items)
                o = x2
                P.tt(o[:, 0:512], pa, x2[:, 0:512], ALU.add)
                P.tt(o[:, 512:1024], pb, x2[:, 512:1024], ALU.add)
                P.dma("sp", dst_d[t * 128:(t + 1) * 128, :], o, o)
        P.barrier()
        P.sb_off = m

    def ffn(layer, src_d, dst_d, w1s, w3s, w2s, moe):
        m = P.sb_off
        if moe:
            compute_mod(layer, 1, nffn_d)
        nexp = len(w1s)
        TB = 1024
        hTs = [P.tile([128, 8, TB], BF16, "fhT0")]
        off_ov = P.sb_off
        if not moe:
            hTs.append(P.tile([128, 8, TB], BF16, "fhT1"))
        actT = P.tile([128, NF, TB], BF16, "actT")
        W2 = P.tile([128, NF, D], BF16, "W2")
        NWB = 4
        W1g = [P.tile([128, 8, 256], BF16, f"W1g{i}") for i in range(NWB)]
        W3g = [P.tile([128, 8, 256], BF16, f"W3g{i}") for i in range(NWB)]
        xts = [P.tile([128, D], F32, f"fx{i}") for i in range(2)]
        junk = P.tile([128, D], F32, "fjunk")
        hbf = P.tile([128, D], BF16, "fhbf")
        ss = P.tile([128, 1], F32, "fss")
        ms = P.tile([128, 1], F32, "fms")
        rstd = P.tile([128, 1], F32, "frstd")
        sg = [P.tile([128, 512], BF16, f"sg{i}") for i in range(2)]
        xr = [P.tile([128, D], F32, f"fxr{i}") for i in range(2)]
        ytmp = junk
        if moe:
            acc = P.tile([128, 8, D], F32, "acc")
            h32 = P.tile([128, D], F32, "h32")
            hT32 = P.tile([128, 8, 128], F32, "hT32")
            rw = P.tile([128, 8, NE], F32, "rw")
            lg = P.tile([128, NE], F32, "lg")
            lg2 = P.tile([128, NE], F32, "lg2")
            mk1 = P.tile([128, NE], F32, "mk1")
            mk2 = P.tile([128, NE], F32, "mk2")
            m1 = P.tile([128, 1], F32, "m1")
            m2 = P.tile([128, 1], F32, "m2")
            dd = P.tile([128, 1], F32, "dd")
            w1g = P.tile([128, 1], F32, "w1g")
            w2g = P.tile([128, 1], F32, "w2g")
            gates = P.tile([128, 8, NE], F32, "gates")
            P.dma("sp", rw, rw_d[0].rearrange("(k p) e -> p k e", p=128), rw)
        nblk = max(1, nt_run // 8)
        gi = 0
        hbf2 = [hbf, P.tile([128, D], BF16, "fhbf2")]

        def norm_a(blk, ti):
            t = blk * 8 + ti
            xt = xts[t % 2]
            hb = hbf2[ti % 2]
            P.dma("sp", xt, src_d[t * 128:(t + 1) * 128, :], xt)
            P.act(junk, xt, AF.Square, accum=ss)
            P.ts(ms, ss, 1.0 / D, EPS, ALU.mult, ALU.add)
            P.rsqrt(rstd, ms, nhalf[:, 0:1])
            P.stt(junk, xt, rstd, modb[:, 0, :], ALU.mult, ALU.mult)
            P.tt(hb, junk, modb[:, 1, :], ALU.add)

        def norm_b(blk, ti, bank):
            hb = hbf2[ti % 2]
            tp = PS[bank].bitcast(BF16)
            P.pe([("tr", tp[:, j * 128:(j + 1) * 128], hb[:, j * 128:(j + 1) * 128], ident) for j in range(8)])
            P.copy(hTs[blk % len(hTs)][:, :, ti * 128:(ti + 1) * 128], tp.re("p (k t) -> p k t", k=8), eng="act")

        def load_chunk(ci):
            if ci >= nblk * (NF // 2):
                return
            fgc = ci % (NF // 2)
            wa_, wb_ = W1g[ci % NWB], W3g[ci % NWB]
            P.dma("pool", wa_.re("p k c -> p (k c)"), w1s[0][fgc * 128:(fgc + 1) * 128, :], wa_)
            P.dma("pool", wb_.re("p k c -> p (k c)"), w3s[0][fgc * 128:(fgc + 1) * 128, :], wb_)

        if not moe:
            P.dma("pool", W2.re("p f n -> p (f n)"), w2s[0], W2)
            for pj in range(NWB - 1):
                load_chunk(pj)
            save_off = P.sb_off
            P.sb_off = off_ov
            compute_mod(layer, 1, nffn_d)
            P.sb_off = save_off
            for ti in range(8):
                norm_a(0, ti)
                if ti >= 1:
                    norm_b(0, ti - 1, 0)
            norm_b(0, 7, 0)
        for blk in range(nblk):
            hT = hTs[blk % len(hTs)]
            for ti in range(8 if moe else 0):
                t = blk * 8 + ti
                xt = xts[t % 2]
                P.dma("sp", xt, src_d[t * 128:(t + 1) * 128, :], xt)
                norm_tile(xt, hT[:, :, ti * 128:(ti + 1) * 128], (junk, ss, ms, rstd, hbf, PS[0]),
                          h32=h32 if moe else None)
                if moe:
                    for hh in range(2):
                        P.pe([("tr", PS[1 + hh][:, g * 128:(g + 1) * 128],
                               h32[:, (hh * 4 + g) * 128:(hh * 4 + g + 1) * 128], ident32) for g in range(4)])
                        P.copy(hT32[:, hh * 4:hh * 4 + 4, :], PS[1 + hh].re("p (k t) -> p k t", k=4), eng="act")
                    P.pe([("mm", PS[3][:, 0:NE], hT32[:, k, :], rw[:, k, :], k == 0, k == 7) for k in range(8)])
                    P.copy(lg, PS[3][:, 0:NE])
                    P.reduce(m1, lg, op=ALU.max)
                    P.ts(mk1, lg, m1, None, ALU.is_ge)
                    P.stt(lg2, mk1, -1e30, lg, ALU.mult, ALU.add)
                    P.reduce(m2, lg2, op=ALU.max)
                    P.ts(mk2, lg2, m2, None, ALU.is_ge)
                    P.tt(dd, m2, m1, ALU.subtract)
                    P.act(dd, dd, AF.Exp)
                    P.ts(dd, dd, 1.0, None, ALU.add)
                    P.recip(w1g, dd)
                    P.ts(w2g, w1g, -1.0, 1.0, ALU.mult, ALU.add)
                    P.ts(mk1, mk1, w1g, None, ALU.mult)
                    P.stt(gates[:, ti, :], mk2, w2g, mk1, ALU.mult, ALU.add)
            for e in range(nexp):
                w1d, w3d, w2d = w1s[e], w3s[e], w2s[e]
                if moe:
                    P.dma("pool", W2, w2d.rearrange("(f p) n -> p f n", p=128), W2)
                elif blk > 0:
                    P.dma("pool", W2.re("p f n -> p (f n)"), w2d, W2)
                if not moe:
                    P.tt(W2, W2, modb[:, 2, :].unsq(1).bc([128, NF, D]), ALU.mult)
                for fg in range(NF // 2):
                    wa, wb = W1g[gi % NWB], W3g[gi % NWB]
                    if moe:
                        P.dma("pool", wa, w1d[:, fg * 256:(fg + 1) * 256].rearrange("(k p) n -> p k n", p=128), wa)
                        P.dma("pool", wb, w3d[:, fg * 256:(fg + 1) * 256].rearrange("(k p) n -> p k n", p=128), wb)
                    else:
                        load_chunk(gi + NWB - 1)
                    gi += 1
                    for fi in range(2):
                        f = fg * 2 + fi
                        for sub in range(2):
                            gp = PS[(2 * sub) % 4]
                            up = PS[(2 * sub + 1) % 4]
                            tok = slice(sub * 512, (sub + 1) * 512)
                            P.pe([("mm", gp, wa[:, k, fi * 128:(fi + 1) * 128], hT[:, k, tok], k == 0, k == 7)
                                  for k in range(8)])
                            P.pe([("mm", up, wb[:, k, fi * 128:(fi + 1) * 128], hT[:, k, tok], k == 0, k == 7)
                                  for k in range(8)])
                            s_ = sg[sub]
                            P.act(s_, gp, AF.Silu)
                            P.tt(actT[:, f, tok], s_, up, ALU.mult)
                    if (not moe) and blk + 1 < nblk:
                        if 1 <= fg <= 8:
                            norm_b(blk + 1, fg - 1, 7)
                        if fg < 8:
                            norm_a(blk + 1, fg)
                for ti in range(8):
                    t = blk * 8 + ti
                    c0 = ti * 128
                    pa, pb = PS[4 + 2 * (ti % 2)], PS[5 + 2 * (ti % 2)]
                    items = []
                    for f in range(NF):
                        l = actT[:, f, c0:c0 + 128]
                        items.append(("mm", pa, l, W2[:, f, 0:512], f == 0, f == NF - 1))
                        items.append(("mm", pb, l, W2[:, f, 512:1024], f == 0, f == NF - 1))
                    P.pe(items)
                    if moe:
                        gcol = gates[:, ti, e:e + 1]
                        if e == 0:
                            P.ts(acc[:, ti, 0:512], pa, gcol, None, ALU.mult)
                            P.ts(acc[:, ti, 512:1024], pb, gcol, None, ALU.mult)
                        else:
                            P.stt(acc[:, ti, 0:512], pa, gcol, acc[:, ti, 0:512], ALU.mult, ALU.add)
                            P.stt(acc[:, ti, 512:1024], pb, gcol, acc[:, ti, 512:1024], ALU.mult, ALU.add)
                    if (not moe) or e == nexp - 1:
                        x2 = xr[t % 2]
                        if ti == 0:
                            P.dma("sp", x2, src_d[t * 128:(t + 1) * 128, :], x2)
                        if ti < 7:
                            P.dma("sp", xr[(t + 1) % 2], src_d[(t + 1) * 128:(t + 2) * 128, :], xr[(t + 1) % 2])
                        o = x2
                        if moe:
                            P.tt(ytmp, acc[:, ti, :], modb[:, 2, :], ALU.mult, eng="pool")
                            P.tt(o, ytmp, x2, ALU.add, eng="pool")
                        else:
                            P.tt(o[:, 0:512], pa, x2[:, 0:512], ALU.add)
                            P.tt(o[:, 512:1024], pb, x2[:, 512:1024], ALU.add)
                        P.dma("sp", dst_d[t * 128:(t + 1) * 128, :], o, o)
        P.barrier()
        P.sb_off = m

    def moe_sparse(layer, src_d, dst_d):
        m = P.sb_off
        compute_mod(layer, 1, nffn_d)
        TB = 768
        TPG = TB // 128
        NG = 18
        ESTR = 6 * TB
        xts = [P.tile([128, D], F32, f"sx{i}") for i in range(2)]
        hbfs = [P.tile([128, D], BF16, f"shbf{i}") for i in range(3)]
        ysb = [P.tile([128, D], F32, f"sys{i}") for i in range(4)]
        gw = P.tile([128, NT, 2], F32, "sgw")
        ridx = P.tile([128, NT, 2], I32, "sridx")
        iH = P.tile([128, NG, TPG], I32, "siH")
        iW = P.tile([128, NG, 11], I32, "siW")
        iW2 = P.tile([128, NG, 2], I32, "siW2")
        mark = P.sb_off
        rw = P.tile([128, 8, NE], F32, "srw")
        runc = P.tile([128, NE], F32, "srunc")
        ebase = P.tile([128, NE], F32, "sebase")
        ustr = P.tile([128, 128], BF16, "sustr")
        giota = P.tile([128, NG], F32, "sgiota")
        cth = P.tile([128, 8], F32, "scth")
        ctw = P.tile([128, 11], F32, "sctw")
        ctw2 = P.tile([128, 2], F32, "sctw2")
        ng = P.tile([128, NE], F32, "sng")
        cum = P.tile([128, NE], F32, "scum")
        tmp8 = P.tile([128, NE], F32, "stmp8")
        E16 = P.tile([128, NG], F32, "sE16")
        O16 = P.tile([128, NG], F32, "sO16")
        t16 = P.tile([128, NG], F32, "st16")
        B16 = P.tile([128, NG], F32, "sB16")
        fH = P.tile([128, NG, TPG], F32, "sfH")
        fW = P.tile([128, NG, 11], F32, "sfW")
        fW2 = P.tile([128, NG, 2], F32, "sfW2")

        def s1_set(i):
            d = {}
            d["junk"] = P.tile([128, D], F32, f"sjunk{i}")
            d["h32"] = P.tile([128, D], F32, f"sh32{i}")
            d["hT32"] = P.tile([128, 8, 128], F32, f"shT32{i}")
            for nm in ("ss", "ms", "rstd", "m1", "m2", "dd"):
                d[nm] = P.tile([128, 1], F32, f"s{nm}{i}")
            for nm in ("lg", "lg2", "mk1", "mk2", "rk", "rk2"):
                d[nm] = P.tile([128, NE], F32, f"s{nm}{i}")
            d["mk12"] = P.tile([128, NE], BF16, f"smk12{i}")
            d["rf"] = P.tile([128, 2], F32, f"srf{i}")
            d["banks"] = (PS[1], PS[2], PS[3], PS[4]) if i != 1 else (PS[5], PS[6], PS[7], PS[0])
            return d
        sets = [s1_set(0), s1_set(1), s1_set(2)]
        P.dma("sp", rw, rw_d[0].rearrange("(k p) e -> p k e", p=128), rw)
        P.dma("sp", ebase, ebase_d, ebase)
        P.dma("sp", giota, giota_d, giota)
        P.dma("sp", cth, cth_d, cth)
        P.dma("sp", ctw, ctw_d, ctw)
        P.dma("sp", ctw2, ctw2_d, ctw2)
        P.tt(ustr, tri, ident, ALU.subtract)
        P.memset(runc, 0.0)

        def g_route(t):
            d = sets[t % 3]
            junk, h32, hT32 = d["junk"], d["h32"], d["hT32"]
            ss, ms, rstd, m1, m2, dd = d["ss"], d["ms"], d["rstd"], d["m1"], d["m2"], d["dd"]
            lg, lg2, mk1, mk2, rk, rk2, mk12, rf = d["lg"], d["lg2"], d["mk1"], d["mk2"], d["rk"], d["rk2"], d["mk12"], d["rf"]
            bt0, bt1, blg, brk = d["banks"]
            xt = xts[t % 2]
            hbf = hbfs[t % 3]
            P.dma("sp", xt, src_d[t * 128:(t + 1) * 128, :], xt); yield
            P.act(junk, xt, AF.Square, accum=ss); yield
            P.ts(ms, ss, 1.0 / D, EPS, ALU.mult, ALU.add); yield
            P.rsqrt(rstd, ms, nhalf[:, 0:1]); yield
            P.stt(h32, xt, rstd, modb[:, 0, :], ALU.mult, ALU.mult); yield
            P.tt(h32, h32, modb[:, 1, :], ALU.add); yield
            P.copy(hbf, h32, eng="act"); yield
            for hh, bk in ((0, bt0), (1, bt1)):
                P.pe([("tr", bk[:, g * 128:(g + 1) * 128],
                       h32[:, (hh * 4 + g) * 128:(hh * 4 + g + 1) * 128], ident32) for g in range(4)]); yield
                P.copy(hT32[:, hh * 4:hh * 4 + 4, :], bk.re("p (k t) -> p k t", k=4), eng="act"); yield
            P.pe([("mm", blg[:, 0:NE], hT32[:, k, :], rw[:, k, :], k == 0, k == 7) for k in range(8)]); yield
            P.copy(lg, blg[:, 0:NE]); yield
            P.reduce(m1, lg, op=ALU.max); yield
            P.ts(mk1, lg, m1, None, ALU.is_ge); yield
            P.stt(lg2, mk1, -1e30, lg, ALU.mult, ALU.add); yield
            P.reduce(m2, lg2, op=ALU.max); yield
            P.ts(mk2, lg2, m2, None, ALU.is_ge); yield
            P.tt(dd, m2, m1, ALU.subtract); yield
            P.act(dd, dd, AF.Exp); yield
            P.ts(dd, dd, 1.0, None, ALU.add); yield
            P.recip(gw[:, t, 0:1], dd); yield
            P.ts(gw[:, t, 1:2], gw[:, t, 0:1], -1.0, 1.0, ALU.mult, ALU.add); yield
            P.tt(mk12, mk1, mk2, ALU.add); yield
            P.pe([("mm", brk[:, 0:NE], ustr, mk12, True, True),
                  ("mm", brk[:, NE:2 * NE], ones, mk12, True, True)]); yield
            P.tt(rk, brk[:, 0:NE], runc, ALU.add)
            P.tt(runc, runc, brk[:, NE:2 * NE], ALU.add); yield
            P.tt(rk, rk, ebase, ALU.add); yield
            P.stt(rk2, rk, 1.0, mk1, ALU.mult, ALU.mult, accum=rf[:, 0:1]); yield
            P.stt(rk2, rk, 1.0, mk2, ALU.mult, ALU.mult, accum=rf[:, 1:2]); yield
            P.copy(ridx[:, t, :], rf); yield
            P.idma(hs_d, hbf, ridx[:, t, 0:1], hbf, True); yield
            P.idma(hs_d, hbf, ridx[:, t, 1:2], hbf, True); yield

        pend = []
        for t in range(nt_run):
            pend.append(g_route(t))
            for _ in range(13):
                for g in list(pend):
                    try:
                        next(g)
                    except StopIteration:
                        pend.remove(g)
        while pend:
            for g in list(pend):
                try:
                    next(g)
                except StopIteration:
                    pend.remove(g)

        P.memset(ng, 0.0)
        for jj in range(6):
            P.ts(tmp8, runc, float(jj * TB) + 0.5, None, ALU.is_gt)
            P.tt(ng, ng, tmp8, ALU.add)
        P.copy(cum, ng)
        for e in range(1, NE):
            P.tt(cum[:, e:e + 1], cum[:, e:e + 1], cum[:, e - 1:e], ALU.add)
        P.memset(E16, 0.0)
        P.copy(O16, giota)
        for e in range(NE):
            P.ts(t16, giota, cum[:, e:e + 1], None, ALU.is_ge)
            P.tt(E16, E16, t16, ALU.add)
            P.ts(t16, t16, ng[:, e:e + 1], None, ALU.mult)
            P.tt(O16, O16, t16, ALU.subtract)
        P.ts(E16, E16, float(NE - 1), None, ALU.min)
        P.ts(O16, O16, 5.0, 0.0, ALU.min, ALU.max)
        P.ts(B16, E16, float(ESTR), None, ALU.mult)
        P.stt(B16, O16, float(TB), B16, ALU.mult, ALU.add)
        P.tt(fH, B16.unsq(2).bc([128, NG, TPG]), cth[:, 0:TPG].unsq(1).bc([128, NG, TPG]), ALU.add)
        P.copy(iH, fH)
        P.ts(t16, E16, float(11 * 128), None, ALU.mult)
        P.tt(fW, t16.unsq(2).bc([128, NG, 11]), ctw.unsq(1).bc([128, NG, 11]), ALU.add)
        P.copy(iW, fW)
        P.ts(t16, E16, 256.0, None, ALU.mult)
        P.tt(fW2, t16.unsq(2).bc([128, NG, 2]), ctw2.unsq(1).bc([128, NG, 2]), ALU.add)
        P.copy(iW2, fW2)
        P.barrier()

        P.sb_off = mark
        hTs = [P.tile([128, 8, TB], BF16, f"shT{i}") for i in range(2)]
        actT = P.tile([128, NF, TB], BF16, "sactT")
        W2 = P.tile([128, NF, D], BF16, "sW2")
        NWB = 4
        W1g = [P.tile([128, 8, 256], BF16, f"sW1g{i}") for i in range(NWB)]
        W3g = [P.tile([128, 8, 256], BF16, f"sW3g{i}") for i in range(NWB)]
        sg = [P.tile([128, 512], BF16, f"ssg{i}") for i in range(2)]
        ngroups = NG if n_tiles_dbg is None else max(1, n_tiles_dbg // 2)
        gi = 0
        W2f = W2.re("p f n -> p (f n)")

        def slot_a(g, ti):
            hsl = hbfs[ti % 2]
            P.idma(hsl, hs_d, iH[:, g, ti:ti + 1], hsl, False)

        def slot_b(g, ti, bank):
            hsl = hbfs[ti % 2]
            tp = PS[bank].bitcast(BF16)
            P.pe([("tr", tp[:, j * 128:(j + 1) * 128], hsl[:, j * 128:(j + 1) * 128], ident) for j in range(8)])
            P.copy(hTs[g % 2][:, :, ti * 128:(ti + 1) * 128], tp.re("p (k t) -> p k t", k=8), eng="act")

        for g in range(ngroups):
            for half in range(2):
                P.idma(W2f[:, half * 11 * D:(half + 1) * 11 * D], w2L_d, iW2[:, g, half:half + 1], W2, False)
            hT = hTs[g % 2]
            if g == 0:
                for ti in range(TPG):
                    slot_a(0, ti)
                    slot_b(0, ti, ti % 2)
            for fg in range(NF // 2):
                wa, wb = W1g[gi % NWB], W3g[gi % NWB]
                gi += 1
                P.idma(wa.re("p k c -> p (k c)"), w1L_d, iW[:, g, fg:fg + 1], wa, False)
                P.idma(wb.re("p k c -> p (k c)"), w3L_d, iW[:, g, fg:fg + 1], wb, False)
                for fi in range(2):
                    f = fg * 2 + fi
                    for sub in range(2):
                        gp = PS[(2 * sub) % 4]
                        up = PS[(2 * sub + 1) % 4]
                        SW = TB // 2
                        tok = slice(sub * SW, (sub + 1) * SW)
                        P.pe([("mm", gp[:, 0:SW], wa[:, k, fi * 128:(fi + 1) * 128], hT[:, k, tok], k == 0, k == 7)
                              for k in range(8)])
                        P.pe([("mm", up[:, 0:SW], wb[:, k, fi * 128:(fi + 1) * 128], hT[:, k, tok], k == 0, k == 7)
                              for k in range(8)])
                        s_ = sg[sub]
                        P.act(s_[:, 0:SW], gp[:, 0:SW], AF.Silu)
                        P.tt(actT[:, f, tok], s_[:, 0:SW], up[:, 0:SW], ALU.mult)
                if g + 1 < ngroups:
                    if 1 <= fg <= TPG:
                        slot_b(g + 1, fg - 1, 7)
                    if fg < TPG:
                        slot_a(g + 1, fg)
            for ti in range(TPG):
                c0 = ti * 128
                pa, pb = PS[4 + 2 * (ti % 2)], PS[5 + 2 * (ti % 2)]
                items = []
                for f in range(NF):
                    l = actT[:, f, c0:c0 + 128]
                    items.append(("mm", pa, l, W2[:, f, 0:512], f == 0, f == NF - 1))
                    items.append(("mm", pb, l, W2[:, f, 512:1024], f == 0, f == NF - 1))
                P.pe(items)
                yb = ysb[ti % 2]
                P.copy(yb[:, 0:512], pa, eng="act")
                P.copy(yb[:, 512:1024], pb)
                P.idma(ys_d, yb, iH[:, g, ti:ti + 1], yb, True)
        P.barrier()

        def s4_loads(t):
            xt = xts[t % 2]
            y0, y1 = ysb[2 * (t % 2)], ysb[2 * (t % 2) + 1]
            P.dma("sp", xt, src_d[t * 128:(t + 1) * 128, :], xt)
            P.idma(y0, ys_d, ridx[:, t, 0:1], y0, False)
            P.idma(y1, ys_d, ridx[:, t, 1:2], y1, False)

        s4_loads(0)
        for t in range(nt_run):
            xt = xts[t % 2]
            y0, y1 = ysb[2 * (t % 2)], ysb[2 * (t % 2) + 1]
            if t + 1 < nt_run:
                s4_loads(t + 1)
            P.ts(y0, y0, gw[:, t, 0:1], None, ALU.mult)
            P.stt(y0, y1, gw[:, t, 1:2], y0, ALU.mult, ALU.add)
            P.tt(y0, y0, modb[:, 2, :], ALU.mult)
            P.tt(xt, y0, xt, ALU.add)
            P.dma("sp", dst_d[t * 128:(t + 1) * 128, :], xt, xt)
        P.barrier()
        P.sb_off = m

    phases = [
        ("mix0", lambda s, d: mixer(0, s, d)),
        ("ffn0", lambda s, d: ffn(0, s, d, [fw1L_d], [fw3L_d], [fw2L_d], False)),
        ("mix1", lambda s, d: mixer(1, s, d)),
        ("ffn1", (lambda s, d: moe_sparse(1, s, d)) if SPARSE else
         (lambda s, d: ffn(1, s, d, [moe_w1_d[0, e] for e in range(NE)],
                           [moe_w3_d[0, e] for e in range(NE)],
                           [moe_w2_d[0, e] for e in range(NE)], True))),
    ]
    if stop_after == "ffn1only":
        phases = phases[3:]
    elif stop_after is not None:
        phases = phases[:[p[0] for p in phases].index(stop_after) + 1]
    for i, (name, fn) in enumerate(phases):
        src = x_d if i == 0 else xs_d
        dst = y_d if i == len(phases) - 1 else xs_d
        fn(src, dst)
    P.barrier()
    P.finish()
    return nc, stack


def make_in_maps(inputs, cores):
    f = np.float32
    A = lambda a: np.ascontiguousarray(a)
    bc = lambda a: A(np.broadcast_to(a[:, None, :], (a.shape[0], 128, a.shape[1])))
    inv_freq = (1.0 / (np.float32(10000.0) ** (np.arange(0, 64, 2, dtype=f) / f(64)))).astype(f)
    shared = {
        "invf": A(np.broadcast_to(inv_freq[None, :], (128, 32))),
        "ident": np.eye(128, dtype=f),
        "tri": A(np.triu(np.ones((128, 128), dtype=f))),
        "ada_w": A(inputs["ada_w"]), "ada_b": A(inputs["ada_b"]),
        "nmix_b": bc(inputs["norm_mix"]), "nffn_b": bc(inputs["norm_ffn"]),
        "w_in": A(inputs["w_in"]),
        "sgn_b": bc(inputs["sgu_norm"]),
        "sguT": A(np.transpose(inputs["sgu_w"], (0, 3, 1, 2))),
        "sgb": A(np.repeat(np.transpose(inputs["sgu_b"], (0, 2, 1)), 64, axis=2)),
        "qln_b": bc(inputs["q_lat_norm"]), "kvln_b": bc(inputs["kv_lat_norm"]),
        "w_uq": A(inputs["w_uq"]), "w_ukv": A(inputs["w_ukv"]),
        "qn_b": bc(inputs["q_norm"]), "kn_b": bc(inputs["k_norm"]),
        "w_out": A(inputs["w_out"]),
        "fw1L": A(inputs["ffn_w1"][0].reshape(8, 128, 11, 256).transpose(2, 1, 0, 3).reshape(11 * 128, 2048)),
        "fw3L": A(inputs["ffn_w3"][0].reshape(8, 128, 11, 256).transpose(2, 1, 0, 3).reshape(11 * 128, 2048)),
        "fw2L": A(inputs["ffn_w2"][0].reshape(NF, 128, D).transpose(1, 0, 2).reshape(128, NF * D)),
        "router_w": A(inputs["router_w"]),
    }
    def lay13(w):
        return A(w[0].reshape(NE, 8, 128, 11, 256).transpose(0, 3, 2, 1, 4).reshape(NE * 11 * 128, 8 * 256))
    shared["w1L"] = lay13(inputs["moe_w1"])
    shared["w3L"] = lay13(inputs["moe_w3"])
    shared["w2L"] = A(inputs["moe_w2"][0].reshape(NE, 2, 11, 128, D).transpose(0, 3, 1, 2, 4).reshape(NE * 128 * 2, 11 * D))
    pp = np.arange(128, dtype=f)[:, None]
    shared["ebase"] = A(np.broadcast_to((np.arange(NE, dtype=f) * 4608)[None, :], (128, NE)))
    shared["giota"] = A(np.broadcast_to(np.arange(18, dtype=f)[None, :], (128, 18)))
    shared["ct_h"] = A(np.arange(8, dtype=f)[None, :] * 128 + pp)
    shared["ct_w"] = A(np.arange(11, dtype=f)[None, :] * 128 + pp)
    shared["ct_w2"] = A(np.arange(2, dtype=f)[None, :] + 2 * pp)
    maps = []
    for b in cores:
        mp = dict(shared)
        mp["x"] = A(inputs["x"][b])
        mp["cT"] = A(inputs["c"][b].reshape(8, 128).T)
        mp["pos"] = A(inputs["positions"][b].reshape(NT, 128).T.astype(np.int32))
        maps.append(mp)
    return maps


def kernel(**inputs):
    inputs = {k: np.asarray(v) for k, v in inputs.items()}
    nc, stack = build_program()
    with stack:
        in_maps = make_in_maps(inputs, list(range(8)))
        res = run_bass_kernel_spmd(nc, in_maps, core_ids=list(range(8)))
    return np.stack([np.asarray(r["y"], dtype=np.float32) for r in res.results], axis=0)
```
